# Optimizing a Trainium2 kernel written in Bass

```python
import jax, jax.numpy as jnp
from jax import lax
import numpy as np

D_MODEL = 1024
BATCH = 16
SEQ = 2048
DEPTH = 1

RET_HEADS = 4
RET_DK = 256
RET_DV = 512
RET_QK_W = RET_HEADS * RET_DK
RET_V_W = RET_HEADS * RET_DV
RET_CHUNK = 128
ROPE_BASE = 10000.0
POOL_WINDOWS = (2, 4, 8, 16)
POOL_GROUPS = 4
POOL_GROUP_W = 256
POOL_W = POOL_GROUPS * POOL_GROUP_W
IN_SIZES = (RET_QK_W, RET_QK_W, RET_V_W, RET_V_W, POOL_W, D_MODEL, D_MODEL)
IN_W = sum(IN_SIZES)
D_FF = 2816
N_MOD = 9
EPS = 1e-6

kernel_name = "hybrid_retention_pool_macaron_adaln"


def rms_norm(x, w):
    xf = x.astype(jnp.float32)
    y = xf * lax.rsqrt(jnp.mean(xf * xf, axis=-1, keepdims=True) + EPS)
    return (y * w.astype(jnp.float32)).astype(x.dtype)


def modulate(h, shift, scale):
    return h * (1 + scale[:, None, :]) + shift[:, None, :]


def swiglu(h, w13, w2):
    a, b = jnp.split(h @ w13, 2, axis=-1)
    return (jax.nn.silu(a) * b) @ w2


def rope(x):
    S, D = x.shape[1], x.shape[-1]
    half = D // 2
    inv = 1.0 / (ROPE_BASE ** (jnp.arange(half, dtype=jnp.float32) / half))
    ang = jnp.arange(S, dtype=jnp.float32)[:, None] * inv[None, :]
    cos = jnp.cos(ang)[None, :, None, :]
    sin = jnp.sin(ang)[None, :, None, :]
    x1, x2 = x[..., :half], x[..., half:]
    return jnp.concatenate([x1 * cos - x2 * sin, x1 * sin + x2 * cos], axis=-1)


def retention_chunkwise(q, k, v):
    B, S, H, DK = q.shape
    DV = v.shape[-1]
    C = RET_CHUNK
    N = S // C
    log_gamma = jnp.log1p(-(2.0 ** (-5.0 - jnp.arange(H, dtype=jnp.float32))))
    idx = jnp.arange(C, dtype=jnp.float32)
    diff = idx[:, None] - idx[None, :]
    inner_decay = jnp.where(diff >= 0, jnp.exp(log_gamma[:, None, None] * jnp.maximum(diff, 0.0)), 0.0)
    q_decay = jnp.exp(log_gamma[:, None] * (idx + 1.0))[None, :, :, None]
    k_decay = jnp.exp(log_gamma[:, None] * (C - 1.0 - idx))[None, :, :, None]
    chunk_decay = jnp.exp(log_gamma * C)[None, :, None, None]

    def to_chunks(t):
        return t.reshape(B, N, C, H, t.shape[-1]).transpose(1, 0, 3, 2, 4)

    qc, kc, vc = to_chunks(q), to_chunks(k), to_chunks(v)

    def step(state, xs):
        qi, ki, vi = xs
        scores = jnp.einsum('bhid,bhjd->bhij', qi, ki) * inner_decay
        inner = jnp.einsum('bhij,bhje->bhie', scores, vi)
        cross = jnp.einsum('bhid,bhde->bhie', qi * q_decay, state)
        new_state = chunk_decay * state + jnp.einsum('bhjd,bhje->bhde', ki * k_decay, vi)
        return new_state, inner + cross

    state0 = jnp.zeros((B, H, DK, DV), jnp.float32)
    _, out = lax.scan(step, state0, (qc, kc, vc))
    return out.transpose(1, 0, 3, 2, 4).reshape(B, S, H, DV)


def head_group_norm(y, w):
    B, S, H, DV = y.shape
    mu = jnp.mean(y, axis=-1, keepdims=True)
    yc = y - mu
    var = jnp.mean(yc * yc, axis=-1, keepdims=True)
    return (yc * lax.rsqrt(var + EPS)).reshape(B, S, H * DV) * w.astype(jnp.float32)


def causal_multiscale_pool(u, lin_w, scale):
    B, S, _ = u.shape
    uf = u.astype(jnp.float32).reshape(B, S, POOL_GROUPS, POOL_GROUP_W)
    cs = jnp.cumsum(uf, axis=1)
    count = jnp.arange(1, S + 1, dtype=jnp.float32)
    outs = []
    for g, w in enumerate(POOL_WINDOWS):
        csg = cs[:, :, g]
        lagged = jnp.pad(csg[:, :S - w], ((0, 0), (w, 0), (0, 0)))
        mean = (csg - lagged) / jnp.minimum(count, float(w))[None, :, None]
        outs.append(mean - uf[:, :, g])
    pooled = jnp.stack(outs, axis=2)
    mixed = jnp.einsum('bsgc,gcd->bsgd', pooled, lin_w.astype(jnp.float32))
    return (mixed.reshape(B, S, POOL_W) * scale.astype(jnp.float32)).astype(u.dtype)


def setup_inputs(seed: int = 0) -> dict:
    key = jax.random.key(seed)
    ks = jax.random.split(key, 20)
    f = jnp.float32

    def nrm(k, shape, s):
        return jax.random.normal(k, shape, f) * s

    L, D = DEPTH, D_MODEL
    return {
        "x": nrm(ks[0], (BATCH, SEQ, D), 1.0),
        "c": nrm(ks[1], (BATCH, D), 1.0),
        "ada_w": nrm(ks[2], (L, D, N_MOD * D), 0.5 * D ** -0.5),
        "ada_b": nrm(ks[3], (L, N_MOD * D), 0.01),
        "norm_ffn1": 1.0 + nrm(ks[4], (L, D), 0.02),
        "ffn1_w13": nrm(ks[5], (L, D, 2 * D_FF), D ** -0.5),
        "ffn1_w2": nrm(ks[6], (L, D_FF, D), D_FF ** -0.5),
        "norm_mix": 1.0 + nrm(ks[7], (L, D), 0.02),
        "w_in": nrm(ks[8], (L, D, IN_W), D ** -0.5),
        "ret_gn_w": 1.0 + nrm(ks[9], (L, RET_V_W), 0.02),
        "w_ret_branch": nrm(ks[10], (L, RET_V_W, D), RET_V_W ** -0.5),
        "pool_lin": nrm(ks[11], (L, POOL_GROUPS, POOL_GROUP_W, POOL_GROUP_W), POOL_GROUP_W ** -0.5),
        "pool_scale": 1.0 + nrm(ks[12], (L, POOL_W), 0.1),
        "w_pool_branch": nrm(ks[13], (L, POOL_W, D), POOL_W ** -0.5),
        "w_out": nrm(ks[14], (L, D, D), D ** -0.5),
        "norm_ffn2": 1.0 + nrm(ks[15], (L, D), 0.02),
        "ffn2_w13": nrm(ks[16], (L, D, 2 * D_FF), D ** -0.5),
        "ffn2_w2": nrm(ks[17], (L, D_FF, D), D_FF ** -0.5),
        "norm_final": 1.0 + nrm(ks[18], (D,), 0.02),
    }


def reference(x, c, ada_w, ada_b, norm_ffn1, ffn1_w13, ffn1_w2, norm_mix, w_in, ret_gn_w,
              w_ret_branch, pool_lin, pool_scale, w_pool_branch, w_out, norm_ffn2, ffn2_w13,
              ffn2_w2, norm_final):
    B, S, D = x.shape
    c_act = jax.nn.silu(c)
    split_pts = np.cumsum(IN_SIZES)[:-1].tolist()
    for l in range(DEPTH):
        mod = c_act @ ada_w[l] + ada_b[l]
        (sh1, sc1, g1, sh2, sc2, g2, sh3, sc3, g3) = jnp.split(mod, N_MOD, axis=-1)

        h = modulate(rms_norm(x, norm_ffn1[l]), sh1, sc1)
        x = x + g1[:, None, :] * (0.5 * swiglu(h, ffn1_w13[l], ffn1_w2[l]))

        h = modulate(rms_norm(x, norm_mix[l]), sh2, sc2)
        proj = h @ w_in[l]
        q, k, v, g, u, a_r, a_p = jnp.split(proj, split_pts, axis=-1)

        qh = rope(q.reshape(B, S, RET_HEADS, RET_DK).astype(jnp.float32))
        kh = rope(k.reshape(B, S, RET_HEADS, RET_DK).astype(jnp.float32)) * (RET_DK ** -0.5)
        vh = v.reshape(B, S, RET_HEADS, RET_DV).astype(jnp.float32)
        ret = head_group_norm(retention_chunkwise(qh, kh, vh), ret_gn_w[l]).astype(x.dtype)
        y_r = (jax.nn.silu(g) * ret) @ w_ret_branch[l]

        y_p = causal_multiscale_pool(u, pool_lin[l], pool_scale[l]) @ w_pool_branch[l]

        merged = jax.nn.sigmoid(a_r) * y_r + jax.nn.sigmoid(a_p) * y_p
        x = x + g2[:, None, :] * (merged @ w_out[l])

        h = modulate(rms_norm(x, norm_ffn2[l]), sh3, sc3)
        x = x + g3[:, None, :] * (0.5 * swiglu(h, ffn2_w13[l], ffn2_w2[l]))
    return rms_norm(x, norm_final)
```

```python
import numpy as np
from contextlib import ExitStack
import concourse.bass as bass
import concourse.mybir as mybir
from concourse.bass_utils import run_bass_kernel_spmd

F32 = mybir.dt.float32
BF16 = mybir.dt.bfloat16
U8 = mybir.dt.uint8
AF = mybir.ActivationFunctionType
ALU = mybir.AluOpType

NCORES = 8
D = 1024
SEQ = 2048
BPC = 2
T = 512
NT = SEQ // T
DFF = 2816
NS = DFF // 128
INW = 9216
EPS = 1e-6
GR = 512
NRING = 4
RING_B = 8192

def _consts():
    half = 128
    inv = (1.0 / (10000.0 ** (np.arange(half, dtype=np.float32) / half))).astype(np.float32)
    pos = np.arange(SEQ, dtype=np.float32)
    ang = (pos[None, :] * inv[:, None]).astype(np.float32)
    cosT = np.cos(ang.astype(np.float64)).astype(np.float32)
    sinT = np.sin(ang.astype(np.float64)).astype(np.float32)
    lg = np.log1p(-(2.0 ** (-5.0 - np.arange(4, dtype=np.float64))))
    idx = np.arange(128, dtype=np.float64)
    ident = np.eye(128, dtype=np.float32)
    mask = np.zeros((128, 4, 128), np.float64)
    for h in range(4):
        d = idx[None, :] - idx[:, None]
        mask[:, h, :] = np.where(d >= 0, np.exp(lg[h] * np.maximum(d, 0.0)), 0.0) / 16.0
    kdec = np.zeros((128, 8), np.float64)
    for h in range(4):
        kdec[:, h] = np.exp(lg[h] * (127.0 - idx)) / 16.0
    qdec = np.zeros((128, 8, 128), np.float64)
    for fc in range(8):
        qdec[:, fc, :] = np.exp(lg[fc // 2] * (idx + 1.0))[None, :]
    rcfix = np.zeros((128, 4, 16), np.float64)
    for g, w in enumerate((2, 4, 8, 16)):
        rcfix[:, g, :] = (1.0 / np.minimum(np.arange(16) + 1.0, float(w)))[None, :]
    cst = np.concatenate([ident, mask.reshape(128, 512), kdec, qdec.reshape(128, 1024), rcfix.reshape(128, 64)],
                         axis=1).astype(np.float32)
    cdec = [float(np.exp(lg[h] * 128.0)) for h in range(4)]
    return cosT, sinT, cst, cdec

C_ID, C_MASK, C_KDEC, C_QDEC, C_RC, C_END = 0, 128, 640, 648, 1672, 1736


class Ref:
    __slots__ = ("ap", "gr")

    def __init__(self, ap, gr):
        self.ap = ap
        self.gr = gr


class SB:
    def __init__(self, arena, off, shape, dt, parts=128):
        self.off = off
        self.shape = tuple(shape)
        self.esz = {BF16: 2, F32: 4, U8: 1}[dt]
        n = int(np.prod(shape))
        self.nbytes = n * self.esz
        ap = arena[0:parts, off:off + self.nbytes]
        if dt != U8:
            ap = ap.bitcast(dt)
        if len(shape) == 2:
            ap = ap.rearrange("p (a b) -> p a b", a=shape[0])
        elif len(shape) == 3:
            ap = ap.rearrange("p (a b c) -> p a b c", a=shape[0], b=shape[1])
        self.full = ap
        st = []
        acc = 1
        for s in reversed(self.shape):
            st.append(acc)
            acc *= s
        self.strides = tuple(reversed(st))

    def __getitem__(self, idx):
        if not isinstance(idx, tuple):
            idx = (idx,)
        lo = hi = 0
        for d, size in enumerate(self.shape):
            i = idx[d] if d < len(idx) else slice(None)
            if isinstance(i, int):
                a = b = i
            else:
                a = i.start or 0
                b = (size if i.stop is None else i.stop) - 1
            lo += a * self.strides[d]
            hi += b * self.strides[d]
        ap = self.full[(slice(None),) + idx]
        g0 = (self.off + lo * self.esz) // GR
        g1 = (self.off + hi * self.esz + self.esz - 1) // GR
        return Ref(ap, [("s", g) for g in range(g0, g1 + 1)])

    def all(self):
        return self[tuple(slice(None) for _ in self.shape)]


class PS:
    def __init__(self, t, i):
        self.t = t
        self.i = i

    def f(self, c0=0, c1=512):
        return Ref(self.t[:, c0:c1], [("p", self.i)])

    def h(self, c0=0, c1=1024):
        return Ref(self.t[:, :].bitcast(BF16)[:, c0:c1], [("p", self.i)])


class Op:
    __slots__ = ("eng", "fn", "deps", "signal", "ticket", "dma", "idx")

    def __init__(self, eng, fn):
        self.eng = eng
        self.fn = fn
        self.deps = []
        self.signal = False
        self.ticket = 0
        self.dma = None


class Builder:
    ENGS = ("pe", "act", "dve", "pool", "sp")
    INORDER = ("pe", "act", "dve", "pool")

    def __init__(self):
        self.ops = {e: [] for e in self.ENGS}
        self.last_w = {}
        self.readers = {}
        self.slot_cnt = {}
        self.nops = 0

    def add(self, eng, fn, reads=(), writes=(), slot=None, ndma=1):
        op = Op(eng, fn)
        op.idx = self.nops
        self.nops += 1
        deps = {}
        for r in reads:
            for g in r.gr:
                w = self.last_w.get(g)
                if w is not None:
                    deps[id(w)] = w
                if g[0] == "p":
                    for rd in self.readers.get(g, ()):
                        if rd.eng != eng:
                            deps[id(rd)] = rd
        for wr in writes:
            for g in wr.gr:
                w = self.last_w.get(g)
                if w is not None:
                    deps[id(w)] = w
                for rd in self.readers.get(g, ()):
                    deps[id(rd)] = rd
        deps.pop(id(op), None)
        op.deps = list(deps.values())
        for wr in writes:
            for g in wr.gr:
                self.last_w[g] = op
                self.readers[g] = []
        for r in reads:
            for g in r.gr:
                lst = self.readers.setdefault(g, [])
                if op.dma is None and slot is None and lst and lst[-1].eng == eng and lst[-1].dma is None and eng in self.INORDER:
                    lst[-1] = op
                else:
                    lst.append(op)
        if slot is not None:
            v = self.slot_cnt.get(slot, 0) + 16 * ndma
            self.slot_cnt[slot] = v
            op.dma = (slot, v)
        self.ops[eng].append(op)
        return op

    @staticmethod
    def _a(x):
        return x.ap if isinstance(x, Ref) else x

    @staticmethod
    def _r(*xs):
        return [x for x in xs if isinstance(x, Ref)]

    def mm(self, out, lhsT, rhs, start, stop):
        rd = [lhsT, rhs] + ([] if start else [out])
        return self.add("pe", lambda e: e.matmul(out.ap, lhsT=lhsT.ap, rhs=rhs.ap, start=start, stop=stop),
                        rd, [out])

    def tr(self, out, in_, ident):
        return self.add("pe", lambda e: e.transpose(out.ap, in_.ap, ident.ap), [in_, ident], [out])

    def act(self, out, in_, func, scale=1.0, bias=0.0, accum=None):
        def fn(e):
            kw = {}
            if accum is not None:
                kw["accum_out"] = accum.ap
            return e.activation(out=out.ap, in_=in_.ap, func=func, bias=self._a(bias), scale=self._a(scale), **kw)
        return self.add("act", fn, self._r(in_, scale, bias), self._r(out, accum))

    def tt(self, eng, out, a, b, op):
        return self.add(eng, lambda e: e.tensor_tensor(out=out.ap, in0=a.ap, in1=b.ap, op=op), [a, b], [out])

    def stt(self, eng, out, in0, scalar, in1, op0, op1):
        return self.add(eng, lambda e: e.scalar_tensor_tensor(out=out.ap, in0=in0.ap, scalar=self._a(scalar),
                                                               in1=in1.ap, op0=op0, op1=op1),
                        self._r(in0, scalar, in1), [out])

    def ts(self, eng, out, in0, s1, s2, op0, op1=None):
        def fn(e):
            if op1 is None:
                return e.tensor_scalar(out=out.ap, in0=in0.ap, scalar1=self._a(s1), scalar2=None, op0=op0)
            return e.tensor_scalar(out=out.ap, in0=in0.ap, scalar1=self._a(s1), scalar2=self._a(s2), op0=op0, op1=op1)
        return self.add(eng, fn, self._r(in0, s1, s2), [out])

    def copy(self, eng, out, in_):
        if eng == "act":
            return self.add("act", lambda e: e.copy(out=out.ap, in_=in_.ap), [in_], [out])
        return self.add(eng, lambda e: e.tensor_copy(out=out.ap, in_=in_.ap), [in_], [out])

    def recip(self, out, in_):
        return self.add("dve", lambda e: e.reciprocal(out=out.ap, in_=in_.ap), [in_], [out])

    def memset(self, eng, out, val):
        return self.add(eng, lambda e: e.memset(out.ap, val), [], [out])

    def finalize(self):
        for e in self.ENGS:
            for op in self.ops[e]:
                keep = []
                for d in op.deps:
                    if d.dma is None and d.eng == "pe" and op.eng == "pe" and op.dma is None:
                        continue
                    keep.append(d)
                    if d.dma is None:
                        d.signal = True
                op.deps = keep
        for e in self.INORDER:
            n = 0
            for op in self.ops[e]:
                if op.dma is None and op.signal:
                    n += 1
                    op.ticket = n

    def emit(self, engname, e, sems, slot_sems):
        waited = {}
        nwait = 0
        for op in self.ops[engname]:
            need = {}
            for d in op.deps:
                if d.dma is not None:
                    key, val = ("slot", d.dma[0]), d.dma[1]
                else:
                    key, val = ("eng", d.eng), d.ticket
                if val > need.get(key, 0):
                    need[key] = val
            for key, val in need.items():
                if waited.get(key, 0) >= val:
                    continue
                waited[key] = val
                sem = slot_sems[key[1]] if key[0] == "slot" else sems[key[1]]
                e.wait_ge(sem, val)
                nwait += 1
            if op.dma is not None:
                op.fn(e, slot_sems[op.dma[0]])
            else:
                ins = op.fn(e)
                if op.signal:
                    ins.then_inc(sems[engname], 1)
        return nwait


def build_program(stage=None, ntiles=None):
    cosT_np, sinT_np, cst_np, cdec = _consts()
    nc = bass.Bass("TRN2", target_bir_lowering=False)
    K = Builder()

    def din(name, shape, dt=F32):
        return nc.dram_tensor(name, list(shape), dt, kind="ExternalInput").ap()

    x_d = din("x", [BPC * SEQ, D])
    c16_d = din("c16", [BPC * 8, 128])
    vecs_d = din("vecs", [128, 128])
    adaw_d = din("ada_w", [D, INW])
    w13_d = [din("ffn1_w13", [D, 2 * DFF]), din("ffn2_w13", [D, 2 * DFF])]
    w2_d = [din("ffn1_w2", [DFF, D]), din("ffn2_w2", [DFF, D])]
    win_d = din("w_in", [D, INW])
    wr_d = din("w_ret", [2048, D])
    pl_d = din("pool_lin", [4, 256, 256])
    wp_d = din("w_pool", [D, D])
    wo_d = din("w_out", [D, D])
    nfin_d = din("norm_final", [1, D])
    cos_d = din("cosT", [128, SEQ])
    sin_d = din("sinT", [128, SEQ])
    cst_d = din("cst", [128, C_END])
    out_d = nc.dram_tensor("out", [BPC * SEQ, D], F32, kind="ExternalOutput").ap()
    TOT = 210944
    dbg_d = nc.dram_tensor("dbg", [128, TOT], U8, kind="ExternalOutput").ap() if stage else None

    def scr(name, shape):
        return nc.dram_tensor(name, list(shape), BF16, kind="Internal").ap()

    s13 = [scr("s13_0", [11, 128, 2, 8, 256]), scr("s13_1", [11, 128, 2, 8, 256])]
    s2 = [scr("s2_0", [8, 128, NS, 128]), scr("s2_1", [8, 128, NS, 128])]
    sin_s = scr("s_in", [18, 128, 8, 512])
    sr = scr("s_r", [4, 128, 16, 256])
    spl = scr("s_pl", [128, 4, 2, 256])
    sp_s = scr("s_p", [2, 128, 8, 512])
    so_s = scr("s_o", [2, 128, 8, 512])

    es = ExitStack()
    arena = es.enter_context(nc.sbuf_tensor("arena", [128, TOT], U8))
    pst = [es.enter_context(nc.psum_tensor(f"ps{i}", [128, 512], F32)) for i in range(8)]
    P = [PS(pst[i], i) for i in range(8)]

    def sb(off, shape, dt):
        return SB(arena, off, shape, dt)

    xT = sb(0, (8, 512), F32)
    stF = sb(16384, (4, 2, 512), F32)
    stB = sb(32768, (4, 2, 512), BF16)
    cosS = sb(40960, (SEQ,), F32)
    sinS = sb(49152, (SEQ,), F32)
    cst = sb(57344, (C_END,), F32)
    wfin = sb(64512, (D,), F32)
    hT = sb(68608, (8, 512), BF16)
    M0 = 76800
    identb = sb(M0, (128,), BF16)
    onesb = sb(M0 + 256, (128,), BF16)
    vecT = sb(M0 + 512, (128,), F32)
    cact = sb(M0 + 1024, (2, 8), F32)
    modT = sb(M0 + 1088, (2, 72), F32)
    Asc = sb(M0 + 1664, (3, 2, 8), F32)
    Gsc = sb(M0 + 1856, (3, 2, 8), F32)
    Uh = sb(M0 + 2048, (8, 16), F32)
    t16f = sb(M0 + 2560, (2, 16), F32)
    mv = sb(M0 + 2656, (4, 2), F32)
    rs4 = sb(M0 + 2688, (4,), F32)
    nb4 = sb(M0 + 2704, (4,), F32)
    sq4 = sb(M0 + 2720, (4,), F32)
    ssq = sb(M0 + 2736, (4,), F32)
    S_sb = sb(M0 + 3072, (4, 128), BF16)
    N0 = M0 + 4096
    st6h = [sb(N0 + h * 512, (6,), F32) for h in range(4)]
    mvh = [sb(N0 + 2048 + h * 512, (2,), F32) for h in range(4)]
    sc3 = [sb(N0 + 4096 + h * 512, (3,), F32) for h in range(4)]
    R0 = N0 + 6144
    ring_off = [R0 + i * RING_B for i in range(NRING)]
    A0 = R0 + NRING * RING_B
    assert A0 + 89 * 1024 <= TOT, (A0, TOT)

    def ar(off, shape, dt):
        return sb(A0 + off, shape, dt)

    KB = 1024
    sq = ar(0, (8, 512), BF16)
    rsd = ar(34 * KB, (512,), F32)
    tmpn = [ar(36 * KB, (512,), F32), ar(38 * KB, (512,), F32)]
    sa = [ar(8 * KB, (512,), F32), ar(10 * KB, (512,), F32)]
    hid = ar(12 * KB, (NS, 512), BF16)
    rtmp = [ar(i * 2 * KB, (512,), F32) for i in range(4)]
    on_t = ar(0, (4, 512), BF16)
    kd = ar(4 * KB, (1024,), BF16)
    qd = ar(6 * KB, (8, 128), BF16)
    qT = ar(8 * KB, (8, 512), BF16)
    kT = ar(16 * KB, (8, 512), BF16)
    v_tok = ar(24 * KB, (4, 2048), BF16)
    sgT = ar(40 * KB, (16, 512), BF16)
    gatedT = ar(56 * KB, (16, 512), BF16)
    sig_ar = ar(73 * KB, (8, 512), BF16)
    sig_ap = ar(81 * KB, (8, 512), BF16)
    m_r = ar(0, (8, 512), F32)
    ptmp = [ar(16 * KB, (2, 528), F32), ar(16 * KB + 4224, (2, 528), F32)]
    U = ar(24 * KB + 256, (8, 528), F32)
    pooledT = ar(41 * KB, (8, 512), BF16)
    mixp = ar(57 * KB, (8, 512), BF16)
    merged = ar(16 * KB, (8, 512), BF16)
    m_p = [ar(28 * KB, (512,), F32), ar(30 * KB, (512,), F32)]
    otok = [ar(40 * KB, (D,), F32), ar(44 * KB, (D,), F32)]
    junk = ar(36 * KB, (512,), BF16)
    xtok = [ar(64 * KB + i * 4 * KB, (D,), F32) for i in range(4)]
    xTn = ar(48 * KB, (8, 512), F32)
    adab = [ar(0, (8, 512), F32), ar(16 * KB, (8, 512), F32)]
    vec128 = ar(32 * KB, (128,), F32)
    c16s = SB(arena, A0 + 32 * KB + 512, (128,), F32, parts=16)

    identf = cst[C_ID:C_ID + 128]
    ring_i = [0]
    psi = [0]

    def ps_next():
        p = P[psi[0] % 7]
        psi[0] += 1
        return p

    def dma(queue, out_ap, in_ap, reads, writes, slot, **kw):
        def fn(e, sem):
            e.dma_start(out=out_ap, in_=in_ap, **kw).then_inc(sem, 16)
        return K.add(queue, fn, reads, writes, slot=slot)

    def dma_multi(queue, pairs, reads, writes, slot, **kw):
        def fn(e, sem):
            for (o, i) in pairs:
                e.dma_start(out=o, in_=i, **kw).then_inc(sem, 16)
        return K.add(queue, fn, reads, writes, slot=slot, ndma=len(pairs))

    def dref(name, blk=0):
        return Ref(None, [("d", name, blk)])

    w13v = [w13_d[l].rearrange("(dc p) f -> p dc f", p=128) for l in range(2)]
    w2v = [w2_d[l].rearrange("(s p) d -> p s d", p=128) for l in range(2)]
    winv = win_d.rearrange("(dc p) f -> p dc f", p=128)
    wrv = wr_d.rearrange("(ec p) d -> p ec d", p=128)
    plv = pl_d.rearrange("g (cc p) d -> p g cc d", p=128)
    wpv = wp_d.rearrange("(dc p) f -> p dc f", p=128)
    wov = wo_d.rearrange("(dc p) f -> p dc f", p=128)
    fresh = [True]
    fresh_i = [0]

    def wsrc(name, blk):
        if name.startswith("s13_"):
            l = int(name[-1])
            return s13[l][blk], [((8, 256), w13v[l][:, :, blk * 256:(blk + 1) * 256], (0,)),
                                 ((8, 256), w13v[l][:, :, DFF + blk * 256:DFF + (blk + 1) * 256], (1,))]
        if name.startswith("s2_"):
            l = int(name[-1])
            src = w2v[l][:, :, blk * 128:(blk + 1) * 128]
            return s2[l][blk], [((11, 128), src[:, 0:11, :], (slice(0, 11),)), ((11, 128), src[:, 11:22, :], (slice(11, 22),))]
        if name in ("s_in", "s_p", "s_o"):
            srcv, scrv = {"s_in": (winv, sin_s), "s_p": (wpv, sp_s), "s_o": (wov, so_s)}[name]
            src = srcv[:, :, blk * 512:(blk + 1) * 512]
            return scrv[blk], [((4, 512), src[:, 0:4, :], (slice(0, 4),)), ((4, 512), src[:, 4:8, :], (slice(4, 8),))]
        if name == "s_r":
            src = wrv[:, :, blk * 256:(blk + 1) * 256]
            return sr[blk], [((8, 256), src[:, 0:8, :], (slice(0, 8),)), ((8, 256), src[:, 8:16, :], (slice(8, 16),))]
        if name == "s_pl":
            return spl, [((2, 2, 256), plv[:, 0:2], (slice(0, 2),)), ((2, 2, 256), plv[:, 2:4], (slice(2, 4),))]
        raise KeyError(name)

    def wload(name, blk, shape):
        scr_ap, halves = wsrc(name, blk)
        if fresh[0]:
            k = fresh_i[0] % 2
            fresh_i[0] += 1
            v = sb(ring_off[2 + k], shape, BF16)
            for hi, (hshape, src, vidx) in enumerate(halves):
                st = sb(ring_off[hi], hshape, F32)
                if name == "s_pl":
                    dma_multi("sp", [(st.full[:, g], src[:, g]) for g in range(2)], [], [st.all()], slot=f"stg{hi}")
                else:
                    dma("sp", st.full, src, [], [st.all()], slot=f"stg{hi}")
                K.copy("dve" if hi == 0 else "act", v[vidx], st.all())
            dma("pool", scr_ap, v.full, [v.all()], [dref(name, blk)], slot=f"wst{k}")
            return v
        i = ring_i[0] % NRING
        ring_i[0] += 1
        v = sb(ring_off[i], shape, BF16)
        dma("sp", v.full, scr_ap, [dref(name, blk)], [v.all()], slot=f"ring{i}")
        return v

    dma_multi("sp", [(cst.full, cst_d), (cosS.full, cos_d), (sinS.full, sin_d),
                     (vec128.full, vecs_d), (c16s.full, c16_d),
                     (wfin.full, nfin_d.partition_broadcast(128))],
              [], [cst.all(), cosS.all(), sinS.all(), vec128.all(), c16s.all(), wfin.all()], slot="cload")
    K.copy("dve", identb.all(), identf)
    K.memset("dve", onesb.all(), 1.0 / D)
    K.memset("dve", Uh.all(), 0.0)
    pv = ps_next()
    K.tr(pv.f(0, 128), vec128.all(), identf)
    K.copy("dve", vecT.all(), pv.f(0, 128))
    pc = ps_next()
    K.tr(pc.f(0, 16), c16s.all(), Ref(cst.full[0:16, C_ID:C_ID + 16], identf.gr))
    K.act(Ref(cact.full.rearrange("p a b -> p (a b)"), cact.all().gr), pc.f(0, 16), AF.Silu)
    adv = adaw_d.rearrange("(dc p) f -> p dc f", p=128)
    modrow = SB(arena, A0 + 33 * KB, (INW,), F32, parts=2)
    for blk in range(18):
        ab = adab[blk % 2]
        dma("sp", ab.full, adv[:, :, blk * 512:(blk + 1) * 512], [], [ab.all()], slot=f"ada{blk % 2}")
        pb_ = ps_next()
        prow = Ref(pb_.t[0:2, :], [("p", pb_.i)])
        for dc in range(8):
            K.mm(prow, Ref(cact.full[:, :, dc], cact.all().gr), ab[dc, :], dc == 0, dc == 7)
        K.copy("dve" if blk % 2 else "act", modrow[blk * 512:(blk + 1) * 512], prow)
    pm = ps_next()
    id2 = Ref(cst.full[0:2, C_ID:C_ID + 2], identf.gr)
    for j in range(72):
        K.tr(pm.f(2 * j, 2 * j + 2), modrow[j * 128:(j + 1) * 128], id2)
    pmv = pm.f(0, 144)
    for b in range(BPC):
        K.tt("dve", modT[b, :], Ref(pm.t[:, 0:144].rearrange("p (j b) -> p b j", b=2)[:, b, :], pmv.gr),
             vecT[0:72], ALU.add)
    NW = (72, 80, 88)
    for n in range(3):
        for b in range(BPC):
            K.stt("dve", Asc[n, b, :], modT[b, (3 * n + 1) * 8:(3 * n + 2) * 8], 1.0, vecT[NW[n]:NW[n] + 8],
                  ALU.add, ALU.mult)
            K.ts("dve", Gsc[n, b, :], modT[b, (3 * n + 2) * 8:(3 * n + 3) * 8], 0.5 if n != 1 else 1.0, None, ALU.mult)

    def shift(n, b, dc):
        return modT[b, 3 * n * 8 + dc:3 * n * 8 + dc + 1]

    pend_mm = []

    def flush_mm():
        while pend_mm:
            pend_mm.pop(0)()

    def x_updated(dc):
        K.act(sq[dc, :], xT[dc, :], AF.Square)
        flush_mm()
        pend_mm.append(lambda dc=dc: K.mm(P[7].f(), onesb.all(), sq[dc, :], dc == 0, dc == 7))

    def rms_mod(n, b):
        flush_mm()
        K.act(rsd.all(), P[7].f(), AF.Sqrt, bias=EPS)
        K.recip(rsd.all(), rsd.all())
        for dc in range(8):
            t = tmpn[dc % 2]
            K.tt("dve", t.all(), xT[dc, :], rsd.all(), ALU.mult)
            K.act(hT[dc, :], t.all(), AF.Identity, scale=Asc[n, b, dc:dc + 1], bias=shift(n, b, dc))

    def ffn(l, b, skip_norm=False, mid_hook=None, group_hook=None):
        n = 0 if l == 0 else 2
        if not skip_norm:
            rms_mod(n, b)
        for blk in range(11):
            w = wload(f"s13_{l}", blk, (2, 8, 256))
            for sl in range(2):
                s = 2 * blk + sl
                pa, pb = ps_next(), ps_next()
                for dc in range(8):
                    K.mm(pa.f(), w[0, dc, sl * 128:(sl + 1) * 128], hT[dc, :], dc == 0, dc == 7)
                for dc in range(8):
                    K.mm(pb.f(), w[1, dc, sl * 128:(sl + 1) * 128], hT[dc, :], dc == 0, dc == 7)
                t = sa[s % 2]
                K.act(t.all(), pa.f(), AF.Silu)
                K.tt("dve", hid[s, :], t.all(), pb.f(), ALU.mult)
        if mid_hook is not None:
            mid_hook()
        for ds in range(8):
            w = wload(f"s2_{l}", ds, (NS, 128))
            po = ps_next()
            for s in range(NS):
                K.mm(po.f(), w[s, :], hid[s, :], s == 0, s == NS - 1)
            K.stt("dve", xT[ds, :], po.f(), Gsc[n, b, ds:ds + 1], xT[ds, :], ALU.mult, ALU.add)
            if l == 0:
                x_updated(ds)
            if group_hook is not None:
                group_hook(ds)

    def proj_fm(blk_lo, nblk, consume):
        for bi in range(nblk):
            w = wload("s_in", blk_lo + bi, (8, 512))
            for sl in range(4):
                p = ps_next()
                for dc in range(8):
                    K.mm(p.f(), w[dc, sl * 128:(sl + 1) * 128], hT[dc, :], dc == 0, dc == 7)
                consume(bi * 4 + sl, p)

    mlim = [99]

    def mixer(b, tt):
        first = (tt == 0)
        pos0 = tt * T
        rms_mod(1, b)
        cosv = cosS[pos0:pos0 + T]
        sinv = sinS[pos0:pos0 + T]
        for (blk_lo, dst) in ((0, qT), (2, kT)):
            pend = {}

            def rope(si, p, dst=dst, pend=pend):
                if si % 2 == 0:
                    pend["x1"] = p
                    return
                p1, p2 = pend["x1"], p
                h = si // 2
                K.tt("dve", rtmp[0].all(), p1.f(), cosv, ALU.mult)
                K.tt("dve", rtmp[1].all(), p2.f(), sinv, ALU.mult)
                K.tt("pool", dst[2 * h, :], rtmp[0].all(), rtmp[1].all(), ALU.subtract)
                K.tt("dve", rtmp[2].all(), p1.f(), sinv, ALU.mult)
                K.tt("dve", rtmp[3].all(), p2.f(), cosv, ALU.mult)
                K.tt("pool", dst[2 * h + 1, :], rtmp[2].all(), rtmp[3].all(), ALU.add)
            proj_fm(blk_lo, 2, rope)
        if mlim[0] < 1:
            return
        for nb in range(4):
            w = wload("s_in", 4 + nb, (8, 512))
            for c in range(4):
                p = ps_next()
                for dc in range(8):
                    K.mm(p.f(), hT[dc, c * 128:(c + 1) * 128], w[dc, :], dc == 0, dc == 7)
                K.copy("act", v_tok[c, nb * 512:(nb + 1) * 512], p.f())
        def g_evac(si, p):
            K.act(sgT[si, :], p.f(), AF.Silu)
            K.ts("dve", sgT[si, :], sgT[si, :], vecT[104 + si:105 + si], None, ALU.mult)
        proj_fm(8, 4, g_evac)
        if mlim[0] < 2:
            return
        if first:
            K.memset("pool", stF.all(), 0.0)
            K.memset("pool", stB.all(), 0.0)
        fill_dst = {14: (sig_ar, 0), 15: (sig_ar, 4), 16: (sig_ap, 0), 17: (sig_ap, 4)}

        def filler_slices(blk):
            w = wload("s_in", blk, (8, 512))
            dst, base = fill_dst[blk]
            out = []
            for sl in range(4):
                def one(sl=sl, w=w, dst=dst, base=base):
                    p = ps_next()
                    for dc in range(8):
                        K.mm(p.f(), w[dc, sl * 128:(sl + 1) * 128], hT[dc, :], dc == 0, dc == 7)
                    K.act(dst[base + sl, :], p.f(), AF.Sigmoid)
                out.append(one)
            return out

        for c in range(4):
            cs = slice(c * 128, (c + 1) * 128)
            fl = filler_slices(14 + c)
            pk = ps_next()
            for fc in range(8):
                K.tr(pk.h(fc * 128, (fc + 1) * 128), kT[fc, cs], identb.all())
            for h in range(4):
                K.act(kd[h * 256:(h + 1) * 256], pk.h(h * 256, (h + 1) * 256), AF.Copy, scale=cst[C_KDEC + h:C_KDEC + h + 1])
            K.tt("pool", qd.all(), qT[:, cs],
                 Ref(cst.full[:, C_QDEC:C_QDEC + 1024].rearrange("p (a b) -> p a b", a=8), cst[C_QDEC:C_QDEC + 1024].gr),
                 ALU.mult)
            psc = ps_next()
            for h in range(4):
                for fi in range(2):
                    fc = 2 * h + fi
                    K.mm(psc.f(h * 128, (h + 1) * 128), kT[fc, cs], qT[fc, cs], fi == 0, fi == 1)
            K.tt("dve", Ref(S_sb.full.rearrange("p a b -> p (a b)"), S_sb.all().gr), psc.f(), cst[C_MASK:C_MASK + 512], ALU.mult)
            fl[0]()
            pos_ = []
            for h in range(4):
                po = ps_next()
                pos_.append(po)
                K.mm(po.f(), S_sb[h, :], v_tok[c, h * 512:(h + 1) * 512], True, False)
                for dc in range(2):
                    K.mm(po.f(), qd[2 * h + dc, :], stB[h, dc, :], False, dc == 1)

            def gn_tail(h, pos_=pos_):
                K.recip(sc3[h][1:2], sc3[h][0:1])
                K.stt("dve", sc3[h][2:3], mvh[h][0:1], -1.0, sc3[h][1:2], ALU.mult, ALU.mult)
                K.act(on_t[h, :], pos_[h].f(), AF.Identity, scale=sc3[h][1:2], bias=sc3[h][2:3])
            for h in range(4):
                K.add("dve", (lambda e, h=h, pp=pos_[h]: e.bn_stats(out=st6h[h].all().ap, in_=pp.f().ap)), [pos_[h].f()], [st6h[h].all()])
                K.add("dve", (lambda e, h=h: e.bn_aggr(out=mvh[h].all().ap, in_=st6h[h].all().ap)), [st6h[h].all()], [mvh[h].all()])
                K.act(sc3[h][0:1], mvh[h][1:2], AF.Sqrt, bias=EPS)
                if h >= 1:
                    gn_tail(h - 1)
            gn_tail(3)
            for h in range(4):
                for dc in range(2):
                    pu = ps_next()
                    K.mm(pu.f(), kd[h * 256 + dc * 128:h * 256 + (dc + 1) * 128], v_tok[c, h * 512:(h + 1) * 512], True, True)
                    K.stt("dve", stF[h, dc, :], stF[h, dc, :], cdec[h], pu.f(), ALU.mult, ALU.add)
                    K.copy("act", stB[h, dc, :], stF[h, dc, :])
            fl[1]()
            fl[2]()
            fl[3]()
            for h in range(4):
                pg = ps_next()
                for e4 in range(4):
                    K.tr(pg.h(e4 * 128, (e4 + 1) * 128), on_t[h, e4 * 128:(e4 + 1) * 128], identb.all())
                K.tt("dve", Ref(gatedT.full[:, h * 4:(h + 1) * 4, cs], gatedT[h * 4:(h + 1) * 4, cs].gr),
                     Ref(pg.t[:, :].bitcast(BF16)[:, 0:512].rearrange("p (a b) -> p a b", a=4), pg.h(0, 512).gr),
                     Ref(sgT.full[:, h * 4:(h + 1) * 4, cs], sgT[h * 4:(h + 1) * 4, cs].gr), ALU.mult)
        if mlim[0] < 3:
            return
        if first:
            K.memset("pool", Uh.all(), 0.0)
        K.copy("pool", U[:, 0:16], Uh.all())
        proj_fm(12, 2, lambda si, p: K.copy("act", U[si, 16:528], p.f()))
        K.copy("pool", Uh.all(), U[:, 512:528])
        dve_pool_ops = []
        for g, wdt in enumerate((2, 4, 8, 16)):
            srcgr = U[2 * g:2 * g + 2, :].gr
            cur_ap, cur_gr = U.full[:, 2 * g:2 * g + 2, :], srcgr
            lag = 1
            k = 0
            while lag < wdt:
                dst = ptmp[k % 2]
                k += 1
                K.tt("pool", Ref(dst.full[:, :, lag:528], dst.all().gr),
                     Ref(cur_ap[:, :, lag:528], cur_gr), Ref(cur_ap[:, :, 0:528 - lag], cur_gr), ALU.add)
                cur_ap, cur_gr = dst.full, dst.all().gr
                lag *= 2
            pooled = Ref(pooledT.full[:, 2 * g:2 * g + 2, :], pooledT[2 * g:2 * g + 2, :].gr)
            ug = Ref(U.full[:, 2 * g:2 * g + 2, 16:528], srcgr)
            K.stt("dve", pooled, Ref(cur_ap[:, :, 16:528], cur_gr), 1.0 / wdt, ug, ALU.mult, ALU.subtract)
            if first:
                rc = Ref(cst.full[:, C_RC + g * 16:C_RC + (g + 1) * 16].unsqueeze(1).broadcast_to([128, 2, 16]), cst[C_RC:C_END].gr)
                K.tt("dve", t16f.all(), Ref(cur_ap[:, :, 16:32], cur_gr), rc, ALU.mult)
                K.tt("dve", Ref(pooledT.full[:, 2 * g:2 * g + 2, 0:16], pooled.gr), t16f.all(),
                     Ref(U.full[:, 2 * g:2 * g + 2, 16:32], srcgr), ALU.subtract)
        for blk in range(4):
            w = wload("s_r", blk, (16, 256))
            for dl in range(2):
                ds = blk * 2 + dl
                p = ps_next()
                for ec in range(16):
                    K.mm(p.f(), w[ec, dl * 128:(dl + 1) * 128], gatedT[ec, :], ec == 0, ec == 15)
                K.tt("dve", m_r[ds, :], p.f(), sig_ar[ds, :], ALU.mult)
        if mlim[0] < 4:
            return
        wpl = wload("s_pl", 0, (4, 2, 256))
        for g in range(4):
            for dl in range(2):
                p = ps_next()
                for cc in range(2):
                    K.mm(p.f(), wpl[g, cc, dl * 128:(dl + 1) * 128], pooledT[2 * g + cc, :], cc == 0, cc == 1)
                K.act(mixp[2 * g + dl, :], p.f(), AF.Identity, scale=vecT[120 + 2 * g + dl:121 + 2 * g + dl])
        for blk in range(2):
            w = wload("s_p", blk, (8, 512))
            for dl in range(4):
                ds = blk * 4 + dl
                p = ps_next()
                for kc in range(8):
                    K.mm(p.f(), w[kc, dl * 128:(dl + 1) * 128], mixp[kc, :], kc == 0, kc == 7)
                t = m_p[ds % 2]
                K.tt("dve", t.all(), p.f(), sig_ap[ds, :], ALU.mult)
                K.tt("pool", merged[ds, :], t.all(), m_r[ds, :], ALU.add)
        for blk in range(2):
            w = wload("s_o", blk, (8, 512))
            for dl in range(4):
                ds = blk * 4 + dl
                p = ps_next()
                for kc in range(8):
                    K.mm(p.f(), w[kc, dl * 128:(dl + 1) * 128], merged[kc, :], kc == 0, kc == 7)
                K.stt("dve", xT[ds, :], p.f(), Gsc[1, b, ds:ds + 1], xT[ds, :], ALU.mult, ALU.add)
                x_updated(ds)

    def load_x(b, tt):
        r0 = b * SEQ + tt * T
        dma_multi("sp", [(xtok[c].full, x_d[r0 + c * 128:r0 + (c + 1) * 128, :]) for c in range(4)],
                  [], [xtok[c].all() for c in range(4)], slot="xload")

    def x_to_fm():
        for dc in range(8):
            p = ps_next()
            for c in range(4):
                K.tr(p.f(c * 128, (c + 1) * 128), xtok[c][dc * 128:(dc + 1) * 128], identf)
            K.copy("act" if dc % 2 else "dve", xT[dc, :], p.f())
            x_updated(dc)

    def prep_next_a():
        for dc in range(8):
            p = ps_next()
            for c in range(4):
                K.tr(p.f(c * 128, (c + 1) * 128), xtok[c][dc * 128:(dc + 1) * 128], identf)
            K.copy("act" if dc % 2 else "dve", xTn[dc, :], p.f())
            K.act(sq[dc, :], xTn[dc, :], AF.Square)
            pend_mm.append(lambda dc=dc: K.mm(P[7].f(), onesb.all(), sq[dc, :], dc == 0, dc == 7))

    def prep_next_b(ds, nb_):
        if pend_mm:
            pend_mm.pop(0)()
        if ds == 7:
            flush_mm()
            K.act(rsd.all(), P[7].f(), AF.Sqrt, bias=EPS)
            K.recip(rsd.all(), rsd.all())
            for dc in range(8):
                t = tmpn[dc % 2]
                K.tt("dve", t.all(), xTn[dc, :], rsd.all(), ALU.mult)
                K.act(hT[dc, :], t.all(), AF.Identity, scale=Asc[0, nb_, dc:dc + 1], bias=shift(0, nb_, dc))

    def adopt_next():
        for dc in range(8):
            K.copy("act" if dc % 2 else "dve", xT[dc, :], xTn[dc, :])

    def final_store(b, tt):
        r0 = b * SEQ + tt * T
        for c in range(4):
            cs = slice(c * 128, (c + 1) * 128)
            pf = [ps_next(), ps_next()]
            for dc in range(8):
                K.tr(pf[dc // 4].f((dc % 4) * 128, (dc % 4 + 1) * 128), xT[dc, cs], identf)
            for i in range(2):
                K.act(junk.all(), pf[i].f(), AF.Square, accum=ssq[i:i + 1])
            K.tt("dve", ssq[2:3], ssq[0:1], ssq[1:2], ALU.add)
            K.act(ssq[3:4], ssq[2:3], AF.Sqrt, scale=1.0 / D, bias=EPS)
            K.recip(ssq[3:4], ssq[3:4])
            ot = otok[c % 2]
            for i in range(2):
                K.stt("dve", ot[i * 512:(i + 1) * 512], pf[i].f(), ssq[3:4], wfin[i * 512:(i + 1) * 512], ALU.mult, ALU.mult)
            dma("sp", out_d[r0 + c * 128:r0 + (c + 1) * 128, :], ot.full, [ot.all()], [dref("out", r0 + c)], slot=f"ost{c % 2}")

    tiles = [(b, tt) for b in range(BPC) for tt in range(NT)]
    if ntiles:
        tiles = tiles[:ntiles]
    order = ["pro", "x", "ffn1", "mix", "ffn2", "final"]
    mlim[0] = 99
    if stage and stage.startswith("mix") and len(stage) >= 4:
        mlim[0] = "ABCDE".index(stage[3])
        if len(stage) > 4:
            mlim.append(int(stage[4:]))
        stage = "mix"
    lim = order.index(stage) if stage else 99
    if lim >= 1:
        load_x(*tiles[0])
    for ti, (b, tt) in enumerate(tiles):
        fresh[0] = (ti == 0)
        has_next = ti + 1 < len(tiles)
        pipelined = (lim >= 5)
        if lim >= 1 and (ti == 0 or not pipelined):
            x_to_fm()
        if lim >= 2:
            ffn(0, b, skip_norm=(pipelined and ti > 0))
        if lim >= 3:
            mixer(b, tt)
        if has_next and lim >= 1:
            load_x(*tiles[ti + 1])
        if lim >= 4:
            if has_next and pipelined:
                nb_ = tiles[ti + 1][0]
                ffn(1, b, mid_hook=prep_next_a, group_hook=lambda ds, nb_=nb_: prep_next_b(ds, nb_))
            else:
                ffn(1, b)
        if lim >= 5:
            final_store(b, tt)
            if has_next:
                adopt_next()
    if stage:
        whole = Ref(arena[:, :], [("s", g) for g in range(0, TOT // GR)])
        dma("sp", dbg_d, arena[:, :], [whole], [dref("dbg")], slot="dbgst")

    K.finalize()
    sems = {e: es.enter_context(nc.semaphore("sem_" + e)) for e in K.INORDER}
    slot_sems = {s: es.enter_context(nc.semaphore("sl_" + s)) for s in K.slot_cnt}
    block = es.enter_context(nc.Block())
    stats = {}

    @block.sync
    def _(e):
        stats["sp"] = K.emit("sp", e, sems, slot_sems)
        for sl in ("ost0", "ost1", "dbgst"):
            if sl in slot_sems:
                e.wait_ge(slot_sems[sl], K.slot_cnt[sl])

    @block.tensor
    def _(e):
        stats["pe"] = K.emit("pe", e, sems, slot_sems)

    @block.scalar
    def _(e):
        stats["act"] = K.emit("act", e, sems, slot_sems)

    @block.vector
    def _(e):
        stats["dve"] = K.emit("dve", e, sems, slot_sems)

    @block.gpsimd
    def _(e):
        stats["pool"] = K.emit("pool", e, sems, slot_sems)

    es.close()
    build_program.stats = {k: (len(K.ops[k]), v) for k, v in stats.items()}
    return nc, (cosT_np, sinT_np, cst_np)


_CACHE = {}


def kernel(x, c, ada_w, ada_b, norm_ffn1, ffn1_w13, ffn1_w2, norm_mix, w_in, ret_gn_w, w_ret_branch, pool_lin,
           pool_scale, w_pool_branch, w_out, norm_ffn2, ffn2_w13, ffn2_w2, norm_final):
    f = lambda a: np.ascontiguousarray(np.asarray(a, dtype=np.float32))
    if "nc" not in _CACHE:
        _CACHE["nc"] = build_program()
    nc, (cosT, sinT, cst) = _CACHE["nc"]
    x = f(x)
    c = f(c)
    vecs = np.concatenate([f(ada_b).reshape(72, 128), f(norm_ffn1).reshape(8, 128), f(norm_mix).reshape(8, 128),
                           f(norm_ffn2).reshape(8, 128), f(norm_final).reshape(8, 128), f(ret_gn_w).reshape(16, 128),
                           f(pool_scale).reshape(8, 128)], axis=0)
    shared = {
        "vecs": np.ascontiguousarray(vecs),
        "ada_w": f(ada_w)[0], "ffn1_w13": f(ffn1_w13)[0], "ffn2_w13": f(ffn2_w13)[0],
        "ffn1_w2": f(ffn1_w2)[0], "ffn2_w2": f(ffn2_w2)[0], "w_in": f(w_in)[0], "w_ret": f(w_ret_branch)[0],
        "pool_lin": f(pool_lin)[0], "w_pool": f(w_pool_branch)[0], "w_out": f(w_out)[0],
        "norm_final": f(norm_final).reshape(1, D), "cosT": cosT, "sinT": sinT, "cst": cst,
    }
    in_maps = []
    for i in range(NCORES):
        m = dict(shared)
        m["x"] = np.ascontiguousarray(x[i * BPC:(i + 1) * BPC].reshape(BPC * SEQ, D))
        m["c16"] = np.ascontiguousarray(c[i * BPC:(i + 1) * BPC].reshape(BPC * 8, 128))
        in_maps.append(m)
    res = run_bass_kernel_spmd(nc, in_maps, core_ids=list(range(NCORES)))
    out = np.concatenate([np.asarray(r["out"]).reshape(BPC, SEQ, D) for r in res.results], axis=0)
    return out.astype(np.float32)
```

```python
import numpy as np
from contextlib import ExitStack
import concourse.bass as bass
import concourse.mybir as mybir
from concourse.bass_utils import run_bass_kernel_spmd

F32 = mybir.dt.float32
BF16 = mybir.dt.bfloat16
U8 = mybir.dt.uint8
AF = mybir.ActivationFunctionType
ALU = mybir.AluOpType

NCORES = 8
D = 1024
SEQ = 2048
BPC = 2
T = 512
NT = SEQ // T
DFF = 2816
NS = DFF // 128
INW = 9216
EPS = 1e-6
GR = 512
NRING = 4
RING_B = 8192

def _consts():
    half = 128
    inv = (1.0 / (10000.0 ** (np.arange(half, dtype=np.float32) / half))).astype(np.float32)
    pos = np.arange(SEQ, dtype=np.float32)
    ang = (pos[None, :] * inv[:, None]).astype(np.float32)
    cosT = np.cos(ang.astype(np.float64)).astype(np.float32)
    sinT = np.sin(ang.astype(np.float64)).astype(np.float32)
    lg = np.log1p(-(2.0 ** (-5.0 - np.arange(4, dtype=np.float64))))
    idx = np.arange(128, dtype=np.float64)
    ident = np.eye(128, dtype=np.float32)
    mask = np.zeros((128, 4, 128), np.float64)
    for h in range(4):
        d = idx[None, :] - idx[:, None]
        mask[:, h, :] = np.where(d >= 0, np.exp(lg[h] * np.maximum(d, 0.0)), 0.0) / 16.0
    kdec = np.zeros((128, 8), np.float64)
    for h in range(4):
        kdec[:, h] = np.exp(lg[h] * (127.0 - idx)) / 16.0
    qdec = np.zeros((128, 8, 128), np.float64)
    for fc in range(8):
        qdec[:, fc, :] = np.exp(lg[fc // 2] * (idx + 1.0))[None, :]
    rcfix = np.zeros((128, 4, 16), np.float64)
    for g, w in enumerate((2, 4, 8, 16)):
        rcfix[:, g, :] = (1.0 / np.minimum(np.arange(16) + 1.0, float(w)))[None, :]
    cst = np.concatenate([ident, mask.reshape(128, 512), kdec, qdec.reshape(128, 1024), rcfix.reshape(128, 64)],
                         axis=1).astype(np.float32)
    cdec = [float(np.exp(lg[h] * 128.0)) for h in range(4)]
    return cosT, sinT, cst, cdec

C_ID, C_MASK, C_KDEC, C_QDEC, C_RC, C_END = 0, 128, 640, 648, 1672, 1736


class Ref:
    __slots__ = ("ap", "gr")

    def __init__(self, ap, gr):
        self.ap = ap
        self.gr = gr


class SB:
    def __init__(self, arena, off, shape, dt, parts=128):
        self.off = off
        self.shape = tuple(shape)
        self.esz = {BF16: 2, F32: 4, U8: 1}[dt]
        n = int(np.prod(shape))
        self.nbytes = n * self.esz
        ap = arena[0:parts, off:off + self.nbytes]
        if dt != U8:
            ap = ap.bitcast(dt)
        if len(shape) == 2:
            ap = ap.rearrange("p (a b) -> p a b", a=shape[0])
        elif len(shape) == 3:
            ap = ap.rearrange("p (a b c) -> p a b c", a=shape[0], b=shape[1])
        self.full = ap
        st = []
        acc = 1
        for s in reversed(self.shape):
            st.append(acc)
            acc *= s
        self.strides = tuple(reversed(st))

    def __getitem__(self, idx):
        if not isinstance(idx, tuple):
            idx = (idx,)
        lo = hi = 0
        for d, size in enumerate(self.shape):
            i = idx[d] if d < len(idx) else slice(None)
            if isinstance(i, int):
                a = b = i
            else:
                a = i.start or 0
                b = (size if i.stop is None else i.stop) - 1
            lo += a * self.strides[d]
            hi += b * self.strides[d]
        ap = self.full[(slice(None),) + idx]
        g0 = (self.off + lo * self.esz) // GR
        g1 = (self.off + hi * self.esz + self.esz - 1) // GR
        return Ref(ap, [("s", g) for g in range(g0, g1 + 1)])

    def all(self):
        return self[tuple(slice(None) for _ in self.shape)]


class PS:
    def __init__(self, t, i):
        self.t = t
        self.i = i

    def f(self, c0=0, c1=512):
        return Ref(self.t[:, c0:c1], [("p", self.i)])

    def h(self, c0=0, c1=1024):
        return Ref(self.t[:, :].bitcast(BF16)[:, c0:c1], [("p", self.i)])


class Op:
    __slots__ = ("eng", "fn", "deps", "signal", "ticket", "dma", "idx")

    def __init__(self, eng, fn):
        self.eng = eng
        self.fn = fn
        self.deps = []
        self.signal = False
        self.ticket = 0
        self.dma = None


class Builder:
    ENGS = ("pe", "act", "dve", "pool", "sp")
    INORDER = ("pe", "act", "dve", "pool")

    def __init__(self):
        self.ops = {e: [] for e in self.ENGS}
        self.last_w = {}
        self.readers = {}
        self.slot_cnt = {}
        self.nops = 0

    def add(self, eng, fn, reads=(), writes=(), slot=None, ndma=1):
        op = Op(eng, fn)
        op.idx = self.nops
        self.nops += 1
        deps = {}
        for r in reads:
            for g in r.gr:
                w = self.last_w.get(g)
                if w is not None:
                    deps[id(w)] = w
                if g[0] == "p":
                    for rd in self.readers.get(g, ()):
                        if rd.eng != eng:
                            deps[id(rd)] = rd
        for wr in writes:
            for g in wr.gr:
                w = self.last_w.get(g)
                if w is not None:
                    deps[id(w)] = w
                for rd in self.readers.get(g, ()):
                    deps[id(rd)] = rd
        deps.pop(id(op), None)
        op.deps = list(deps.values())
        for wr in writes:
            for g in wr.gr:
                self.last_w[g] = op
                self.readers[g] = []
        for r in reads:
            for g in r.gr:
                lst = self.readers.setdefault(g, [])
                if op.dma is None and slot is None and lst and lst[-1].eng == eng and lst[-1].dma is None and eng in self.INORDER:
                    lst[-1] = op
                else:
                    lst.append(op)
        if slot is not None:
            v = self.slot_cnt.get(slot, 0) + 16 * ndma
            self.slot_cnt[slot] = v
            op.dma = (slot, v)
        self.ops[eng].append(op)
        return op

    @staticmethod
    def _a(x):
        return x.ap if isinstance(x, Ref) else x

    @staticmethod
    def _r(*xs):
        return [x for x in xs if isinstance(x, Ref)]

    def mm(self, out, lhsT, rhs, start, stop):
        rd = [lhsT, rhs] + ([] if start else [out])
        return self.add("pe", lambda e: e.matmul(out.ap, lhsT=lhsT.ap, rhs=rhs.ap, start=start, stop=stop),
                        rd, [out])

    def tr(self, out, in_, ident):
        return self.add("pe", lambda e: e.transpose(out.ap, in_.ap, ident.ap), [in_, ident], [out])

    def act(self, out, in_, func, scale=1.0, bias=0.0, accum=None):
        def fn(e):
            kw = {}
            if accum is not None:
                kw["accum_out"] = accum.ap
            return e.activation(out=out.ap, in_=in_.ap, func=func, bias=self._a(bias), scale=self._a(scale), **kw)
        return self.add("act", fn, self._r(in_, scale, bias), self._r(out, accum))

    def tt(self, eng, out, a, b, op):
        return self.add(eng, lambda e: e.tensor_tensor(out=out.ap, in0=a.ap, in1=b.ap, op=op), [a, b], [out])

    def stt(self, eng, out, in0, scalar, in1, op0, op1):
        return self.add(eng, lambda e: e.scalar_tensor_tensor(out=out.ap, in0=in0.ap, scalar=self._a(scalar),
                                                               in1=in1.ap, op0=op0, op1=op1),
                        self._r(in0, scalar, in1), [out])

    def ts(self, eng, out, in0, s1, s2, op0, op1=None):
        def fn(e):
            if op1 is None:
                return e.tensor_scalar(out=out.ap, in0=in0.ap, scalar1=self._a(s1), scalar2=None, op0=op0)
            return e.tensor_scalar(out=out.ap, in0=in0.ap, scalar1=self._a(s1), scalar2=self._a(s2), op0=op0, op1=op1)
        return self.add(eng, fn, self._r(in0, s1, s2), [out])

    def copy(self, eng, out, in_):
        if eng == "act":
            return self.add("act", lambda e: e.copy(out=out.ap, in_=in_.ap), [in_], [out])
        return self.add(eng, lambda e: e.tensor_copy(out=out.ap, in_=in_.ap), [in_], [out])

    def recip(self, out, in_):
        return self.add("dve", lambda e: e.reciprocal(out=out.ap, in_=in_.ap), [in_], [out])

    def memset(self, eng, out, val):
        return self.add(eng, lambda e: e.memset(out.ap, val), [], [out])

    def finalize(self):
        for e in self.ENGS:
            for op in self.ops[e]:
                keep = []
                for d in op.deps:
                    if d.dma is None and d.eng == "pe" and op.eng == "pe" and op.dma is None:
                        continue
                    keep.append(d)
                    if d.dma is None:
                        d.signal = True
                op.deps = keep
        for e in self.INORDER:
            n = 0
            for op in self.ops[e]:
                if op.dma is None and op.signal:
                    n += 1
                    op.ticket = n

    def emit(self, engname, e, sems, slot_sems):
        waited = {}
        nwait = 0
        for op in self.ops[engname]:
            need = {}
            for d in op.deps:
                if d.dma is not None:
                    key, val = ("slot", d.dma[0]), d.dma[1]
                else:
                    key, val = ("eng", d.eng), d.ticket
                if val > need.get(key, 0):
                    need[key] = val
            for key, val in need.items():
                if waited.get(key, 0) >= val:
                    continue
                waited[key] = val
                sem = slot_sems[key[1]] if key[0] == "slot" else sems[key[1]]
                e.wait_ge(sem, val)
                nwait += 1
            if op.dma is not None:
                op.fn(e, slot_sems[op.dma[0]])
            else:
                ins = op.fn(e)
                if op.signal:
                    ins.then_inc(sems[engname], 1)
        return nwait


def build_program(stage=None, ntiles=None):
    cosT_np, sinT_np, cst_np, cdec = _consts()
    nc = bass.Bass("TRN2", target_bir_lowering=False)
    K = Builder()

    def din(name, shape, dt=F32):
        return nc.dram_tensor(name, list(shape), dt, kind="ExternalInput").ap()

    x_d = din("x", [BPC * SEQ, D])
    c16_d = din("c16", [BPC * 8, 128])
    vecs_d = din("vecs", [128, 128])
    adaw_d = din("ada_w", [D, INW])
    w13_d = [din("ffn1_w13", [D, 2 * DFF]), din("ffn2_w13", [D, 2 * DFF])]
    w2_d = [din("ffn1_w2", [DFF, D]), din("ffn2_w2", [DFF, D])]
    win_d = din("w_in", [D, INW])
    wr_d = din("w_ret", [2048, D])
    pl_d = din("pool_lin", [4, 256, 256])
    wp_d = din("w_pool", [D, D])
    wo_d = din("w_out", [D, D])
    nfin_d = din("norm_final", [1, D])
    cos_d = din("cosT", [128, SEQ])
    sin_d = din("sinT", [128, SEQ])
    cst_d = din("cst", [128, C_END])
    out_d = nc.dram_tensor("out", [BPC * SEQ, D], F32, kind="ExternalOutput").ap()
    TOT = 210944
    dbg_d = nc.dram_tensor("dbg", [128, TOT], U8, kind="ExternalOutput").ap() if stage else None

    def scr(name, shape):
        return nc.dram_tensor(name, list(shape), BF16, kind="Internal").ap()

    s13 = [scr("s13_0", [11, 128, 2, 8, 256]), scr("s13_1", [11, 128, 2, 8, 256])]
    s2 = [scr("s2_0", [8, 128, NS, 128]), scr("s2_1", [8, 128, NS, 128])]
    sin_s = scr("s_in", [18, 128, 8, 512])
    sr = scr("s_r", [4, 128, 16, 256])
    spl = scr("s_pl", [128, 4, 2, 256])
    sp_s = scr("s_p", [2, 128, 8, 512])
    so_s = scr("s_o", [2, 128, 8, 512])

    es = ExitStack()
    arena = es.enter_context(nc.sbuf_tensor("arena", [128, TOT], U8))
    pst = [es.enter_context(nc.psum_tensor(f"ps{i}", [128, 512], F32)) for i in range(8)]
    P = [PS(pst[i], i) for i in range(8)]

    def sb(off, shape, dt):
        return SB(arena, off, shape, dt)

    xT = sb(0, (8, 512), F32)
    stF = sb(16384, (4, 2, 512), F32)
    stB = sb(32768, (4, 2, 512), BF16)
    cosS = sb(40960, (SEQ,), F32)
    sinS = sb(49152, (SEQ,), F32)
    cst = sb(57344, (C_END,), F32)
    wfin = sb(64512, (D,), F32)
    hT = sb(68608, (8, 512), BF16)
    M0 = 76800
    identb = sb(M0, (128,), BF16)
    onesb = sb(M0 + 256, (128,), BF16)
    vecT = sb(M0 + 512, (128,), F32)
    cact = sb(M0 + 1024, (2, 8), F32)
    modT = sb(M0 + 1088, (2, 72), F32)
    Asc = sb(M0 + 1664, (3, 2, 8), F32)
    Gsc = sb(M0 + 1856, (3, 2, 8), F32)
    Uh = sb(M0 + 2048, (8, 16), F32)
    t16f = sb(M0 + 2560, (2, 16), F32)
    mv = sb(M0 + 2656, (4, 2), F32)
    rs4 = sb(M0 + 2688, (4,), F32)
    nb4 = sb(M0 + 2704, (4,), F32)
    sq4 = sb(M0 + 2720, (4,), F32)
    ssq = sb(M0 + 2736, (4,), F32)
    S_sb = sb(M0 + 3072, (4, 128), BF16)
    N0 = M0 + 4096
    st6h = [sb(N0 + h * 512, (6,), F32) for h in range(4)]
    mvh = [sb(N0 + 2048 + h * 512, (2,), F32) for h in range(4)]
    sc3 = [sb(N0 + 4096 + h * 512, (3,), F32) for h in range(4)]
    R0 = N0 + 6144
    ring_off = [R0 + i * RING_B for i in range(NRING)]
    A0 = R0 + NRING * RING_B
    assert A0 + 89 * 1024 <= TOT, (A0, TOT)

    def ar(off, shape, dt):
        return sb(A0 + off, shape, dt)

    KB = 1024
    sq = ar(0, (8, 512), BF16)
    rsd = ar(34 * KB, (512,), F32)
    tmpn = [ar(36 * KB, (512,), F32), ar(38 * KB, (512,), F32)]
    sa = [ar(8 * KB, (512,), F32), ar(10 * KB, (512,), F32)]
    hid = ar(12 * KB, (NS, 512), BF16)
    rtmp = [ar(i * 2 * KB, (512,), F32) for i in range(4)]
    on_t = ar(0, (4, 512), BF16)
    kd = ar(4 * KB, (1024,), BF16)
    qd = ar(6 * KB, (8, 128), BF16)
    qT = ar(8 * KB, (8, 512), BF16)
    kT = ar(16 * KB, (8, 512), BF16)
    v_tok = ar(24 * KB, (4, 2048), BF16)
    sgT = ar(40 * KB, (16, 512), BF16)
    gatedT = ar(56 * KB, (16, 512), BF16)
    sig_ar = ar(73 * KB, (8, 512), BF16)
    sig_ap = ar(81 * KB, (8, 512), BF16)
    m_r = ar(0, (8, 512), F32)
    ptmp = [ar(16 * KB, (2, 528), F32), ar(16 * KB + 4224, (2, 528), F32)]
    U = ar(24 * KB + 256, (8, 528), F32)
    pooledT = ar(41 * KB, (8, 512), BF16)
    mixp = ar(57 * KB, (8, 512), BF16)
    merged = ar(16 * KB, (8, 512), BF16)
    m_p = [ar(28 * KB, (512,), F32), ar(30 * KB, (512,), F32)]
    otok = [ar(40 * KB, (D,), F32), ar(44 * KB, (D,), F32)]
    junk = ar(36 * KB, (512,), BF16)
    xtok = [ar(64 * KB + i * 4 * KB, (D,), F32) for i in range(4)]
    xTn = ar(48 * KB, (8, 512), F32)
    adab = [ar(0, (8, 512), F32), ar(16 * KB, (8, 512), F32)]
    vec128 = ar(32 * KB, (128,), F32)
    c16s = SB(arena, A0 + 32 * KB + 512, (128,), F32, parts=16)

    identf = cst[C_ID:C_ID + 128]
    ring_i = [0]
    psi = [0]

    def ps_next():
        p = P[psi[0] % 7]
        psi[0] += 1
        return p

    def dma(queue, out_ap, in_ap, reads, writes, slot, **kw):
        def fn(e, sem):
            e.dma_start(out=out_ap, in_=in_ap, **kw).then_inc(sem, 16)
        return K.add(queue, fn, reads, writes, slot=slot)

    def dma_multi(queue, pairs, reads, writes, slot, **kw):
        def fn(e, sem):
            for (o, i) in pairs:
                e.dma_start(out=o, in_=i, **kw).then_inc(sem, 16)
        return K.add(queue, fn, reads, writes, slot=slot, ndma=len(pairs))

    def dref(name, blk=0):
        return Ref(None, [("d", name, blk)])

    w13v = [w13_d[l].rearrange("(dc p) f -> p dc f", p=128) for l in range(2)]
    w2v = [w2_d[l].rearrange("(s p) d -> p s d", p=128) for l in range(2)]
    winv = win_d.rearrange("(dc p) f -> p dc f", p=128)
    wrv = wr_d.rearrange("(ec p) d -> p ec d", p=128)
    plv = pl_d.rearrange("g (cc p) d -> p g cc d", p=128)
    wpv = wp_d.rearrange("(dc p) f -> p dc f", p=128)
    wov = wo_d.rearrange("(dc p) f -> p dc f", p=128)
    fresh = [True]
    fresh_i = [0]

    def wsrc(name, blk):
        if name.startswith("s13_"):
            l = int(name[-1])
            return s13[l][blk], [((8, 256), w13v[l][:, :, blk * 256:(blk + 1) * 256], (0,)),
                                 ((8, 256), w13v[l][:, :, DFF + blk * 256:DFF + (blk + 1) * 256], (1,))]
        if name.startswith("s2_"):
            l = int(name[-1])
            src = w2v[l][:, :, blk * 128:(blk + 1) * 128]
            return s2[l][blk], [((11, 128), src[:, 0:11, :], (slice(0, 11),)), ((11, 128), src[:, 11:22, :], (slice(11, 22),))]
        if name in ("s_in", "s_p", "s_o"):
            srcv, scrv = {"s_in": (winv, sin_s), "s_p": (wpv, sp_s), "s_o": (wov, so_s)}[name]
            src = srcv[:, :, blk * 512:(blk + 1) * 512]
            return scrv[blk], [((4, 512), src[:, 0:4, :], (slice(0, 4),)), ((4, 512), src[:, 4:8, :], (slice(4, 8),))]
        if name == "s_r":
            src = wrv[:, :, blk * 256:(blk + 1) * 256]
            return sr[blk], [((8, 256), src[:, 0:8, :], (slice(0, 8),)), ((8, 256), src[:, 8:16, :], (slice(8, 16),))]
        if name == "s_pl":
            return spl, [((2, 2, 256), plv[:, 0:2], (slice(0, 2),)), ((2, 2, 256), plv[:, 2:4], (slice(2, 4),))]
        raise KeyError(name)

    def wload(name, blk, shape):
        scr_ap, halves = wsrc(name, blk)
        if fresh[0]:
            k = fresh_i[0] % 2
            fresh_i[0] += 1
            v = sb(ring_off[2 + k], shape, BF16)
            for hi, (hshape, src, vidx) in enumerate(halves):
                st = sb(ring_off[hi], hshape, F32)
                if name == "s_pl":
                    dma_multi("sp", [(st.full[:, g], src[:, g]) for g in range(2)], [], [st.all()], slot=f"stg{hi}")
                else:
                    dma("sp", st.full, src, [], [st.all()], slot=f"stg{hi}")
                K.copy("dve" if hi == 0 else "act", v[vidx], st.all())
            dma("pool", scr_ap, v.full, [v.all()], [dref(name, blk)], slot=f"wst{k}")
            return v
        i = ring_i[0] % NRING
        ring_i[0] += 1
        v = sb(ring_off[i], shape, BF16)
        dma("sp", v.full, scr_ap, [dref(name, blk)], [v.all()], slot=f"ring{i}")
        return v

    dma_multi("sp", [(cst.full, cst_d), (cosS.full, cos_d), (sinS.full, sin_d),
                     (vec128.full, vecs_d), (c16s.full, c16_d),
                     (wfin.full, nfin_d.partition_broadcast(128))],
              [], [cst.all(), cosS.all(), sinS.all(), vec128.all(), c16s.all(), wfin.all()], slot="cload")
    K.copy("dve", identb.all(), identf)
    K.memset("dve", onesb.all(), 1.0 / D)
    K.memset("dve", Uh.all(), 0.0)
    pv = ps_next()
    K.tr(pv.f(0, 128), vec128.all(), identf)
    K.copy("dve", vecT.all(), pv.f(0, 128))
    pc = ps_next()
    K.tr(pc.f(0, 16), c16s.all(), Ref(cst.full[0:16, C_ID:C_ID + 16], identf.gr))
    K.act(Ref(cact.full.rearrange("p a b -> p (a b)"), cact.all().gr), pc.f(0, 16), AF.Silu)
    adv = adaw_d.rearrange("(dc p) f -> p dc f", p=128)
    modrow = SB(arena, A0 + 33 * KB, (INW,), F32, parts=2)
    for blk in range(18):
        ab = adab[blk % 2]
        dma("sp", ab.full, adv[:, :, blk * 512:(blk + 1) * 512], [], [ab.all()], slot=f"ada{blk % 2}")
        pb_ = ps_next()
        prow = Ref(pb_.t[0:2, :], [("p", pb_.i)])
        for dc in range(8):
            K.mm(prow, Ref(cact.full[:, :, dc], cact.all().gr), ab[dc, :], dc == 0, dc == 7)
        K.copy("dve" if blk % 2 else "act", modrow[blk * 512:(blk + 1) * 512], prow)
    pm = ps_next()
    id2 = Ref(cst.full[0:2, C_ID:C_ID + 2], identf.gr)
    for j in range(72):
        K.tr(pm.f(2 * j, 2 * j + 2), modrow[j * 128:(j + 1) * 128], id2)
    pmv = pm.f(0, 144)
    for b in range(BPC):
        K.tt("dve", modT[b, :], Ref(pm.t[:, 0:144].rearrange("p (j b) -> p b j", b=2)[:, b, :], pmv.gr),
             vecT[0:72], ALU.add)
    NW = (72, 80, 88)
    for n in range(3):
        for b in range(BPC):
            K.stt("dve", Asc[n, b, :], modT[b, (3 * n + 1) * 8:(3 * n + 2) * 8], 1.0, vecT[NW[n]:NW[n] + 8],
                  ALU.add, ALU.mult)
            K.ts("dve", Gsc[n, b, :], modT[b, (3 * n + 2) * 8:(3 * n + 3) * 8], 0.5 if n != 1 else 1.0, None, ALU.mult)

    def shift(n, b, dc):
        return modT[b, 3 * n * 8 + dc:3 * n * 8 + dc + 1]

    pend_mm = []

    def flush_mm():
        while pend_mm:
            pend_mm.pop(0)()

    def x_updated(dc):
        K.act(sq[dc, :], xT[dc, :], AF.Square)
        flush_mm()
        pend_mm.append(lambda dc=dc: K.mm(P[7].f(), onesb.all(), sq[dc, :], dc == 0, dc == 7))

    def rms_mod(n, b):
        flush_mm()
        K.act(rsd.all(), P[7].f(), AF.Sqrt, bias=EPS)
        K.recip(rsd.all(), rsd.all())
        for dc in range(8):
            t = tmpn[dc % 2]
            K.tt("dve", t.all(), xT[dc, :], rsd.all(), ALU.mult)
            K.act(hT[dc, :], t.all(), AF.Identity, scale=Asc[n, b, dc:dc + 1], bias=shift(n, b, dc))

    def ffn(l, b, skip_norm=False, mid_hook=None, group_hook=None):
        n = 0 if l == 0 else 2
        if not skip_norm:
            rms_mod(n, b)
        for blk in range(11):
            w = wload(f"s13_{l}", blk, (2, 8, 256))
            for sl in range(2):
                s = 2 * blk + sl
                pa, pb = ps_next(), ps_next()
                for dc in range(8):
                    K.mm(pa.f(), w[0, dc, sl * 128:(sl + 1) * 128], hT[dc, :], dc == 0, dc == 7)
                for dc in range(8):
                    K.mm(pb.f(), w[1, dc, sl * 128:(sl + 1) * 128], hT[dc, :], dc == 0, dc == 7)
                t = sa[s % 2]
                K.act(t.all(), pa.f(), AF.Silu)
                K.tt("dve", hid[s, :], t.all(), pb.f(), ALU.mult)
        if mid_hook is not None:
            mid_hook()
        for ds in range(8):
            w = wload(f"s2_{l}", ds, (NS, 128))
            po = ps_next()
            for s in range(NS):
                K.mm(po.f(), w[s, :], hid[s, :], s == 0, s == NS - 1)
            K.stt("dve", xT[ds, :], po.f(), Gsc[n, b, ds:ds + 1], xT[ds, :], ALU.mult, ALU.add)
            if l == 0:
                x_updated(ds)
            if group_hook is not None:
                group_hook(ds)

    def proj_fm(blk_lo, nblk, consume):
        for bi in range(nblk):
            w = wload("s_in", blk_lo + bi, (8, 512))
            for sl in range(4):
                p = ps_next()
                for dc in range(8):
                    K.mm(p.f(), w[dc, sl * 128:(sl + 1) * 128], hT[dc, :], dc == 0, dc == 7)
                consume(bi * 4 + sl, p)

    mlim = [99]

    def mixer(b, tt):
        first = (tt == 0)
        pos0 = tt * T
        rms_mod(1, b)
        cosv = cosS[pos0:pos0 + T]
        sinv = sinS[pos0:pos0 + T]
        for (blk_lo, dst) in ((0, qT), (2, kT)):
            pend = {}

            def rope(si, p, dst=dst, pend=pend):
                if si % 2 == 0:
                    pend["x1"] = p
                    return
                p1, p2 = pend["x1"], p
                h = si // 2
                K.tt("dve", rtmp[0].all(), p1.f(), cosv, ALU.mult)
                K.tt("dve", rtmp[1].all(), p2.f(), sinv, ALU.mult)
                K.tt("pool", dst[2 * h, :], rtmp[0].all(), rtmp[1].all(), ALU.subtract)
                K.tt("dve", rtmp[2].all(), p1.f(), sinv, ALU.mult)
                K.tt("dve", rtmp[3].all(), p2.f(), cosv, ALU.mult)
                K.tt("pool", dst[2 * h + 1, :], rtmp[2].all(), rtmp[3].all(), ALU.add)
            proj_fm(blk_lo, 2, rope)
        if mlim[0] < 1:
            return
        for nb in range(4):
            w = wload("s_in", 4 + nb, (8, 512))
            for c in range(4):
                p = ps_next()
                for dc in range(8):
                    K.mm(p.f(), hT[dc, c * 128:(c + 1) * 128], w[dc, :], dc == 0, dc == 7)
                K.copy("act", v_tok[c, nb * 512:(nb + 1) * 512], p.f())
        def g_evac(si, p):
            K.act(sgT[si, :], p.f(), AF.Silu)
            K.ts("dve", sgT[si, :], sgT[si, :], vecT[104 + si:105 + si], None, ALU.mult)
        proj_fm(8, 4, g_evac)
        if mlim[0] < 2:
            return
        if first:
            K.memset("pool", stF.all(), 0.0)
            K.memset("pool", stB.all(), 0.0)
        fill_dst = {14: (sig_ar, 0), 15: (sig_ar, 4), 16: (sig_ap, 0), 17: (sig_ap, 4)}

        def filler_slices(blk):
            w = wload("s_in", blk, (8, 512))
            dst, base = fill_dst[blk]
            out = []
            for sl in range(4):
                def one(sl=sl, w=w, dst=dst, base=base):
                    p = ps_next()
                    for dc in range(8):
                        K.mm(p.f(), w[dc, sl * 128:(sl + 1) * 128], hT[dc, :], dc == 0, dc == 7)
                    K.act(dst[base + sl, :], p.f(), AF.Sigmoid)
                out.append(one)
            return out

        for c in range(4):
            cs = slice(c * 128, (c + 1) * 128)
            fl = filler_slices(14 + c)
            pk = ps_next()
            for fc in range(8):
                K.tr(pk.h(fc * 128, (fc + 1) * 128), kT[fc, cs], identb.all())
            for h in range(4):
                K.act(kd[h * 256:(h + 1) * 256], pk.h(h * 256, (h + 1) * 256), AF.Copy, scale=cst[C_KDEC + h:C_KDEC + h + 1])
            K.tt("pool", qd.all(), qT[:, cs],
                 Ref(cst.full[:, C_QDEC:C_QDEC + 1024].rearrange("p (a b) -> p a b", a=8), cst[C_QDEC:C_QDEC + 1024].gr),
                 ALU.mult)
            psc = ps_next()
            for h in range(4):
                for fi in range(2):
                    fc = 2 * h + fi
                    K.mm(psc.f(h * 128, (h + 1) * 128), kT[fc, cs], qT[fc, cs], fi == 0, fi == 1)
            K.tt("dve", Ref(S_sb.full.rearrange("p a b -> p (a b)"), S_sb.all().gr), psc.f(), cst[C_MASK:C_MASK + 512], ALU.mult)
            fl[0]()
            pos_ = []
            for h in range(4):
                po = ps_next()
                pos_.append(po)
                K.mm(po.f(), S_sb[h, :], v_tok[c, h * 512:(h + 1) * 512], True, False)
                for dc in range(2):
                    K.mm(po.f(), qd[2 * h + dc, :], stB[h, dc, :], False, dc == 1)

            def gn_tail(h, pos_=pos_):
                K.recip(sc3[h][1:2], sc3[h][0:1])
                K.stt("dve", sc3[h][2:3], mvh[h][0:1], -1.0, sc3[h][1:2], ALU.mult, ALU.mult)
                K.act(on_t[h, :], pos_[h].f(), AF.Identity, scale=sc3[h][1:2], bias=sc3[h][2:3])
            for h in range(4):
                K.add("dve", (lambda e, h=h, pp=pos_[h]: e.bn_stats(out=st6h[h].all().ap, in_=pp.f().ap)), [pos_[h].f()], [st6h[h].all()])
                K.add("dve", (lambda e, h=h: e.bn_aggr(out=mvh[h].all().ap, in_=st6h[h].all().ap)), [st6h[h].all()], [mvh[h].all()])
                K.act(sc3[h][0:1], mvh[h][1:2], AF.Sqrt, bias=EPS)
                if h >= 1:
                    gn_tail(h - 1)
            gn_tail(3)
            for h in range(4):
                for dc in range(2):
                    pu = ps_next()
                    K.mm(pu.f(), kd[h * 256 + dc * 128:h * 256 + (dc + 1) * 128], v_tok[c, h * 512:(h + 1) * 512], True, True)
                    K.stt("dve", stF[h, dc, :], stF[h, dc, :], cdec[h], pu.f(), ALU.mult, ALU.add)
                    K.copy("act", stB[h, dc, :], stF[h, dc, :])
            fl[1]()
            fl[2]()
            fl[3]()
            for h in range(4):
                pg = ps_next()
                for e4 in range(4):
                    K.tr(pg.h(e4 * 128, (e4 + 1) * 128), on_t[h, e4 * 128:(e4 + 1) * 128], identb.all())
                K.tt("dve", Ref(gatedT.full[:, h * 4:(h + 1) * 4, cs], gatedT[h * 4:(h + 1) * 4, cs].gr),
                     Ref(pg.t[:, :].bitcast(BF16)[:, 0:512].rearrange("p (a b) -> p a b", a=4), pg.h(0, 512).gr),
                     Ref(sgT.full[:, h * 4:(h + 1) * 4, cs], sgT[h * 4:(h + 1) * 4, cs].gr), ALU.mult)
        if mlim[0] < 3:
            return
        if first:
            K.memset("pool", Uh.all(), 0.0)
        K.copy("pool", U[:, 0:16], Uh.all())
        proj_fm(12, 2, lambda si, p: K.copy("act", U[si, 16:528], p.f()))
        K.copy("pool", Uh.all(), U[:, 512:528])
        dve_pool_ops = []
        for g, wdt in enumerate((2, 4, 8, 16)):
            srcgr = U[2 * g:2 * g + 2, :].gr
            cur_ap, cur_gr = U.full[:, 2 * g:2 * g + 2, :], srcgr
            lag = 1
            k = 0
            while lag < wdt:
                dst = ptmp[k % 2]
                k += 1
                K.tt("pool", Ref(dst.full[:, :, lag:528], dst.all().gr),
                     Ref(cur_ap[:, :, lag:528], cur_gr), Ref(cur_ap[:, :, 0:528 - lag], cur_gr), ALU.add)
                cur_ap, cur_gr = dst.full, dst.all().gr
                lag *= 2
            pooled = Ref(pooledT.full[:, 2 * g:2 * g + 2, :], pooledT[2 * g:2 * g + 2, :].gr)
            ug = Ref(U.full[:, 2 * g:2 * g + 2, 16:528], srcgr)
            K.stt("dve", pooled, Ref(cur_ap[:, :, 16:528], cur_gr), 1.0 / wdt, ug, ALU.mult, ALU.subtract)
            if first:
                rc = Ref(cst.full[:, C_RC + g * 16:C_RC + (g + 1) * 16].unsqueeze(1).broadcast_to([128, 2, 16]), cst[C_RC:C_END].gr)
                K.tt("dve", t16f.all(), Ref(cur_ap[:, :, 16:32], cur_gr), rc, ALU.mult)
                K.tt("dve", Ref(pooledT.full[:, 2 * g:2 * g + 2, 0:16], pooled.gr), t16f.all(),
                     Ref(U.full[:, 2 * g:2 * g + 2, 16:32], srcgr), ALU.subtract)
        for blk in range(4):
            w = wload("s_r", blk, (16, 256))
            for dl in range(2):
                ds = blk * 2 + dl
                p = ps_next()
                for ec in range(16):
                    K.mm(p.f(), w[ec, dl * 128:(dl + 1) * 128], gatedT[ec, :], ec == 0, ec == 15)
                K.tt("dve", m_r[ds, :], p.f(), sig_ar[ds, :], ALU.mult)
        if mlim[0] < 4:
            return
        wpl = wload("s_pl", 0, (4, 2, 256))
        for g in range(4):
            for dl in range(2):
                p = ps_next()
                for cc in range(2):
                    K.mm(p.f(), wpl[g, cc, dl * 128:(dl + 1) * 128], pooledT[2 * g + cc, :], cc == 0, cc == 1)
                K.act(mixp[2 * g + dl, :], p.f(), AF.Identity, scale=vecT[120 + 2 * g + dl:121 + 2 * g + dl])
        for blk in range(2):
            w = wload("s_p", blk, (8, 512))
            for dl in range(4):
                ds = blk * 4 + dl
                p = ps_next()
                for kc in range(8):
                    K.mm(p.f(), w[kc, dl * 128:(dl + 1) * 128], mixp[kc, :], kc == 0, kc == 7)
                t = m_p[ds % 2]
                K.tt("dve", t.all(), p.f(), sig_ap[ds, :], ALU.mult)
                K.tt("pool", merged[ds, :], t.all(), m_r[ds, :], ALU.add)
        for blk in range(2):
            w = wload("s_o", blk, (8, 512))
            for dl in range(4):
                ds = blk * 4 + dl
                p = ps_next()
                for kc in range(8):
                    K.mm(p.f(), w[kc, dl * 128:(dl + 1) * 128], merged[kc, :], kc == 0, kc == 7)
                K.stt("dve", xT[ds, :], p.f(), Gsc[1, b, ds:ds + 1], xT[ds, :], ALU.mult, ALU.add)
                x_updated(ds)

    def load_x(b, tt):
        r0 = b * SEQ + tt * T
        dma_multi("sp", [(xtok[c].full, x_d[r0 + c * 128:r0 + (c + 1) * 128, :]) for c in range(4)],
                  [], [xtok[c].all() for c in range(4)], slot="xload")

    def x_to_fm():
        for dc in range(8):
            p = ps_next()
            for c in range(4):
                K.tr(p.f(c * 128, (c + 1) * 128), xtok[c][dc * 128:(dc + 1) * 128], identf)
            K.copy("act" if dc % 2 else "dve", xT[dc, :], p.f())
            x_updated(dc)

    def prep_next_a():
        for dc in range(8):
            p = ps_next()
            for c in range(4):
                K.tr(p.f(c * 128, (c + 1) * 128), xtok[c][dc * 128:(dc + 1) * 128], identf)
            K.copy("act" if dc % 2 else "dve", xTn[dc, :], p.f())
            K.act(sq[dc, :], xTn[dc, :], AF.Square)
            pend_mm.append(lambda dc=dc: K.mm(P[7].f(), onesb.all(), sq[dc, :], dc == 0, dc == 7))

    def prep_next_b(ds, nb_):
        for _ in range(2):
            if pend_mm:
                pend_mm.pop(0)()
        if ds == 4:
            flush_mm()
            K.act(rsd.all(), P[7].f(), AF.Sqrt, bias=EPS)
            K.recip(rsd.all(), rsd.all())
            for dc in range(8):
                t = tmpn[dc % 2]
                K.tt("dve", t.all(), xTn[dc, :], rsd.all(), ALU.mult)
                K.act(hT[dc, :], t.all(), AF.Identity, scale=Asc[0, nb_, dc:dc + 1], bias=shift(0, nb_, dc))

    def adopt_next():
        for dc in range(8):
            K.copy("act" if dc % 2 else "dve", xT[dc, :], xTn[dc, :])

    def final_store(b, tt):
        r0 = b * SEQ + tt * T
        for c in range(4):
            cs = slice(c * 128, (c + 1) * 128)
            pf = [ps_next(), ps_next()]
            for dc in range(8):
                K.tr(pf[dc // 4].f((dc % 4) * 128, (dc % 4 + 1) * 128), xT[dc, cs], identf)
            for i in range(2):
                K.act(junk.all(), pf[i].f(), AF.Square, accum=ssq[i:i + 1])
            K.tt("dve", ssq[2:3], ssq[0:1], ssq[1:2], ALU.add)
            K.act(ssq[3:4], ssq[2:3], AF.Sqrt, scale=1.0 / D, bias=EPS)
            K.recip(ssq[3:4], ssq[3:4])
            ot = otok[c % 2]
            for i in range(2):
                K.stt("dve", ot[i * 512:(i + 1) * 512], pf[i].f(), ssq[3:4], wfin[i * 512:(i + 1) * 512], ALU.mult, ALU.mult)
            dma("pool", out_d[r0 + c * 128:r0 + (c + 1) * 128, :], ot.full, [ot.all()], [dref("out", r0 + c)], slot=f"ost{c % 2}")

    tiles = [(b, tt) for b in range(BPC) for tt in range(NT)]
    if ntiles:
        tiles = tiles[:ntiles]
    order = ["pro", "x", "ffn1", "mix", "ffn2", "final"]
    mlim[0] = 99
    if stage and stage.startswith("mix") and len(stage) >= 4:
        mlim[0] = "ABCDE".index(stage[3])
        if len(stage) > 4:
            mlim.append(int(stage[4:]))
        stage = "mix"
    lim = order.index(stage) if stage else 99
    if lim >= 1:
        load_x(*tiles[0])
    for ti, (b, tt) in enumerate(tiles):
        fresh[0] = (ti == 0)
        has_next = ti + 1 < len(tiles)
        pipelined = (lim >= 5)
        if lim >= 1 and (ti == 0 or not pipelined):
            x_to_fm()
        if lim >= 2:
            ffn(0, b, skip_norm=(pipelined and ti > 0))
        if lim >= 3:
            mixer(b, tt)
        if has_next and lim >= 1:
            load_x(*tiles[ti + 1])
        if lim >= 4:
            if has_next and pipelined:
                nb_ = tiles[ti + 1][0]
                ffn(1, b, mid_hook=prep_next_a, group_hook=lambda ds, nb_=nb_: prep_next_b(ds, nb_))
            else:
                ffn(1, b)
        if lim >= 5:
            final_store(b, tt)
            if has_next:
                adopt_next()
    if stage:
        whole = Ref(arena[:, :], [("s", g) for g in range(0, TOT // GR)])
        dma("sp", dbg_d, arena[:, :], [whole], [dref("dbg")], slot="dbgst")

    K.finalize()
    sems = {e: es.enter_context(nc.semaphore("sem_" + e)) for e in K.INORDER}
    slot_sems = {s: es.enter_context(nc.semaphore("sl_" + s)) for s in K.slot_cnt}
    block = es.enter_context(nc.Block())
    stats = {}

    @block.sync
    def _(e):
        stats["sp"] = K.emit("sp", e, sems, slot_sems)
        for sl in ("ost0", "ost1", "dbgst"):
            if sl in slot_sems:
                e.wait_ge(slot_sems[sl], K.slot_cnt[sl])

    @block.tensor
    def _(e):
        stats["pe"] = K.emit("pe", e, sems, slot_sems)

    @block.scalar
    def _(e):
        stats["act"] = K.emit("act", e, sems, slot_sems)

    @block.vector
    def _(e):
        stats["dve"] = K.emit("dve", e, sems, slot_sems)

    @block.gpsimd
    def _(e):
        stats["pool"] = K.emit("pool", e, sems, slot_sems)

    es.close()
    build_program.stats = {k: (len(K.ops[k]), v) for k, v in stats.items()}
    return nc, (cosT_np, sinT_np, cst_np)


_CACHE = {}


def kernel(x, c, ada_w, ada_b, norm_ffn1, ffn1_w13, ffn1_w2, norm_mix, w_in, ret_gn_w, w_ret_branch, pool_lin,
           pool_scale, w_pool_branch, w_out, norm_ffn2, ffn2_w13, ffn2_w2, norm_final):
    f = lambda a: np.ascontiguousarray(np.asarray(a, dtype=np.float32))
    if "nc" not in _CACHE:
        _CACHE["nc"] = build_program()
    nc, (cosT, sinT, cst) = _CACHE["nc"]
    x = f(x)
    c = f(c)
    vecs = np.concatenate([f(ada_b).reshape(72, 128), f(norm_ffn1).reshape(8, 128), f(norm_mix).reshape(8, 128),
                           f(norm_ffn2).reshape(8, 128), f(norm_final).reshape(8, 128), f(ret_gn_w).reshape(16, 128),
                           f(pool_scale).reshape(8, 128)], axis=0)
    shared = {
        "vecs": np.ascontiguousarray(vecs),
        "ada_w": f(ada_w)[0], "ffn1_w13": f(ffn1_w13)[0], "ffn2_w13": f(ffn2_w13)[0],
        "ffn1_w2": f(ffn1_w2)[0], "ffn2_w2": f(ffn2_w2)[0], "w_in": f(w_in)[0], "w_ret": f(w_ret_branch)[0],
        "pool_lin": f(pool_lin)[0], "w_pool": f(w_pool_branch)[0], "w_out": f(w_out)[0],
        "norm_final": f(norm_final).reshape(1, D), "cosT": cosT, "sinT": sinT, "cst": cst,
    }
    in_maps = []
    for i in range(NCORES):
        m = dict(shared)
        m["x"] = np.ascontiguousarray(x[i * BPC:(i + 1) * BPC].reshape(BPC * SEQ, D))
        m["c16"] = np.ascontiguousarray(c[i * BPC:(i + 1) * BPC].reshape(BPC * 8, 128))
        in_maps.append(m)
    res = run_bass_kernel_spmd(nc, in_maps, core_ids=list(range(NCORES)))
    out = np.concatenate([np.asarray(r["out"]).reshape(BPC, SEQ, D) for r in res.results], axis=0)
    return out.astype(np.float32)
```

```python
import numpy as np
from contextlib import ExitStack
import concourse.bass as bass
import concourse.mybir as mybir
from concourse.bass_utils import run_bass_kernel_spmd

F32 = mybir.dt.float32
BF16 = mybir.dt.bfloat16
U8 = mybir.dt.uint8
AF = mybir.ActivationFunctionType
ALU = mybir.AluOpType

NCORES = 8
D = 1024
SEQ = 2048
BPC = 2
T = 512
NT = SEQ // T
DFF = 2816
NS = DFF // 128
INW = 9216
EPS = 1e-6
GR = 512
NRING = 4
RING_B = 8192

def _consts():
    half = 128
    inv = (1.0 / (10000.0 ** (np.arange(half, dtype=np.float32) / half))).astype(np.float32)
    pos = np.arange(SEQ, dtype=np.float32)
    ang = (pos[None, :] * inv[:, None]).astype(np.float32)
    cosT = np.cos(ang.astype(np.float64)).astype(np.float32)
    sinT = np.sin(ang.astype(np.float64)).astype(np.float32)
    lg = np.log1p(-(2.0 ** (-5.0 - np.arange(4, dtype=np.float64))))
    idx = np.arange(128, dtype=np.float64)
    ident = np.eye(128, dtype=np.float32)
    mask = np.zeros((128, 4, 128), np.float64)
    for h in range(4):
        d = idx[None, :] - idx[:, None]
        mask[:, h, :] = np.where(d >= 0, np.exp(lg[h] * np.maximum(d, 0.0)), 0.0) / 16.0
    kdec = np.zeros((128, 8), np.float64)
    for h in range(4):
        kdec[:, h] = np.exp(lg[h] * (127.0 - idx)) / 16.0
    qdec = np.zeros((128, 8, 128), np.float64)
    for fc in range(8):
        qdec[:, fc, :] = np.exp(lg[fc // 2] * (idx + 1.0))[None, :]
    rcfix = np.zeros((128, 4, 16), np.float64)
    for g, w in enumerate((2, 4, 8, 16)):
        rcfix[:, g, :] = (1.0 / np.minimum(np.arange(16) + 1.0, float(w)))[None, :]
    cst = np.concatenate([ident, mask.reshape(128, 512), kdec, qdec.reshape(128, 1024), rcfix.reshape(128, 64)],
                         axis=1).astype(np.float32)
    cdec = [float(np.exp(lg[h] * 128.0)) for h in range(4)]
    return cosT, sinT, cst, cdec

C_ID, C_MASK, C_KDEC, C_QDEC, C_RC, C_END = 0, 128, 640, 648, 1672, 1736


class Ref:
    __slots__ = ("ap", "gr")

    def __init__(self, ap, gr):
        self.ap = ap
        self.gr = gr


class SB:
    def __init__(self, arena, off, shape, dt, parts=128):
        self.off = off
        self.shape = tuple(shape)
        self.esz = {BF16: 2, F32: 4, U8: 1}[dt]
        n = int(np.prod(shape))
        self.nbytes = n * self.esz
        ap = arena[0:parts, off:off + self.nbytes]
        if dt != U8:
            ap = ap.bitcast(dt)
        if len(shape) == 2:
            ap = ap.rearrange("p (a b) -> p a b", a=shape[0])
        elif len(shape) == 3:
            ap = ap.rearrange("p (a b c) -> p a b c", a=shape[0], b=shape[1])
        self.full = ap
        st = []
        acc = 1
        for s in reversed(self.shape):
            st.append(acc)
            acc *= s
        self.strides = tuple(reversed(st))

    def __getitem__(self, idx):
        if not isinstance(idx, tuple):
            idx = (idx,)
        lo = hi = 0
        for d, size in enumerate(self.shape):
            i = idx[d] if d < len(idx) else slice(None)
            if isinstance(i, int):
                a = b = i
            else:
                a = i.start or 0
                b = (size if i.stop is None else i.stop) - 1
            lo += a * self.strides[d]
            hi += b * self.strides[d]
        ap = self.full[(slice(None),) + idx]
        g0 = (self.off + lo * self.esz) // GR
        g1 = (self.off + hi * self.esz + self.esz - 1) // GR
        return Ref(ap, [("s", g) for g in range(g0, g1 + 1)])

    def all(self):
        return self[tuple(slice(None) for _ in self.shape)]


class PS:
    def __init__(self, t, i):
        self.t = t
        self.i = i

    def f(self, c0=0, c1=512):
        return Ref(self.t[:, c0:c1], [("p", self.i)])

    def h(self, c0=0, c1=1024):
        return Ref(self.t[:, :].bitcast(BF16)[:, c0:c1], [("p", self.i)])


class Op:
    __slots__ = ("eng", "fn", "deps", "signal", "ticket", "dma", "idx")

    def __init__(self, eng, fn):
        self.eng = eng
        self.fn = fn
        self.deps = []
        self.signal = False
        self.ticket = 0
        self.dma = None


class Builder:
    ENGS = ("pe", "act", "dve", "pool", "sp")
    INORDER = ("pe", "act", "dve", "pool")

    def __init__(self):
        self.ops = {e: [] for e in self.ENGS}
        self.last_w = {}
        self.readers = {}
        self.slot_cnt = {}
        self.nops = 0

    def add(self, eng, fn, reads=(), writes=(), slot=None, ndma=1):
        op = Op(eng, fn)
        op.idx = self.nops
        self.nops += 1
        deps = {}
        for r in reads:
            for g in r.gr:
                w = self.last_w.get(g)
                if w is not None:
                    deps[id(w)] = w
                if g[0] == "p":
                    for rd in self.readers.get(g, ()):
                        if rd.eng != eng:
                            deps[id(rd)] = rd
        for wr in writes:
            for g in wr.gr:
                w = self.last_w.get(g)
                if w is not None:
                    deps[id(w)] = w
                for rd in self.readers.get(g, ()):
                    deps[id(rd)] = rd
        deps.pop(id(op), None)
        op.deps = list(deps.values())
        for wr in writes:
            for g in wr.gr:
                self.last_w[g] = op
                self.readers[g] = []
        for r in reads:
            for g in r.gr:
                lst = self.readers.setdefault(g, [])
                if op.dma is None and slot is None and lst and lst[-1].eng == eng and lst[-1].dma is None and eng in self.INORDER:
                    lst[-1] = op
                else:
                    lst.append(op)
        if slot is not None:
            v = self.slot_cnt.get(slot, 0) + 16 * ndma
            self.slot_cnt[slot] = v
            op.dma = (slot, v)
        self.ops[eng].append(op)
        return op

    @staticmethod
    def _a(x):
        return x.ap if isinstance(x, Ref) else x

    @staticmethod
    def _r(*xs):
        return [x for x in xs if isinstance(x, Ref)]

    def mm(self, out, lhsT, rhs, start, stop):
        rd = [lhsT, rhs] + ([] if start else [out])
        return self.add("pe", lambda e: e.matmul(out.ap, lhsT=lhsT.ap, rhs=rhs.ap, start=start, stop=stop),
                        rd, [out])

    def tr(self, out, in_, ident):
        return self.add("pe", lambda e: e.transpose(out.ap, in_.ap, ident.ap), [in_, ident], [out])

    def act(self, out, in_, func, scale=1.0, bias=0.0, accum=None):
        def fn(e):
            kw = {}
            if accum is not None:
                kw["accum_out"] = accum.ap
            return e.activation(out=out.ap, in_=in_.ap, func=func, bias=self._a(bias), scale=self._a(scale), **kw)
        return self.add("act", fn, self._r(in_, scale, bias), self._r(out, accum))

    def tt(self, eng, out, a, b, op):
        return self.add(eng, lambda e: e.tensor_tensor(out=out.ap, in0=a.ap, in1=b.ap, op=op), [a, b], [out])

    def stt(self, eng, out, in0, scalar, in1, op0, op1):
        return self.add(eng, lambda e: e.scalar_tensor_tensor(out=out.ap, in0=in0.ap, scalar=self._a(scalar),
                                                               in1=in1.ap, op0=op0, op1=op1),
                        self._r(in0, scalar, in1), [out])

    def ts(self, eng, out, in0, s1, s2, op0, op1=None):
        def fn(e):
            if op1 is None:
                return e.tensor_scalar(out=out.ap, in0=in0.ap, scalar1=self._a(s1), scalar2=None, op0=op0)
            return e.tensor_scalar(out=out.ap, in0=in0.ap, scalar1=self._a(s1), scalar2=self._a(s2), op0=op0, op1=op1)
        return self.add(eng, fn, self._r(in0, s1, s2), [out])

    def copy(self, eng, out, in_):
        if eng == "act":
            return self.add("act", lambda e: e.copy(out=out.ap, in_=in_.ap), [in_], [out])
        return self.add(eng, lambda e: e.tensor_copy(out=out.ap, in_=in_.ap), [in_], [out])

    def recip(self, out, in_):
        return self.add("dve", lambda e: e.reciprocal(out=out.ap, in_=in_.ap), [in_], [out])

    def memset(self, eng, out, val):
        return self.add(eng, lambda e: e.memset(out.ap, val), [], [out])

    def finalize(self):
        for e in self.ENGS:
            for op in self.ops[e]:
                keep = []
                for d in op.deps:
                    if d.dma is None and d.eng == "pe" and op.eng == "pe" and op.dma is None:
                        continue
                    keep.append(d)
                    if d.dma is None:
                        d.signal = True
                op.deps = keep
        for e in self.INORDER:
            n = 0
            for op in self.ops[e]:
                if op.dma is None and op.signal:
                    n += 1
                    op.ticket = n

    def emit(self, engname, e, sems, slot_sems):
        waited = {}
        nwait = 0
        for op in self.ops[engname]:
            need = {}
            for d in op.deps:
                if d.dma is not None:
                    key, val = ("slot", d.dma[0]), d.dma[1]
                else:
                    key, val = ("eng", d.eng), d.ticket
                if val > need.get(key, 0):
                    need[key] = val
            for key, val in need.items():
                if waited.get(key, 0) >= val:
                    continue
                waited[key] = val
                sem = slot_sems[key[1]] if key[0] == "slot" else sems[key[1]]
                e.wait_ge(sem, val)
                nwait += 1
            if op.dma is not None:
                op.fn(e, slot_sems[op.dma[0]])
            else:
                ins = op.fn(e)
                if op.signal:
                    ins.then_inc(sems[engname], 1)
        return nwait


def build_program(stage=None, ntiles=None):
    cosT_np, sinT_np, cst_np, cdec = _consts()
    nc = bass.Bass("TRN2", target_bir_lowering=False)
    K = Builder()

    def din(name, shape, dt=F32):
        return nc.dram_tensor(name, list(shape), dt, kind="ExternalInput").ap()

    x_d = din("x", [BPC * SEQ, D])
    c16_d = din("c16", [BPC * 8, 128])
    vecs_d = din("vecs", [128, 128])
    adaw_d = din("ada_w", [D, INW])
    w13_d = [din("ffn1_w13", [D, 2 * DFF]), din("ffn2_w13", [D, 2 * DFF])]
    w2_d = [din("ffn1_w2", [DFF, D]), din("ffn2_w2", [DFF, D])]
    win_d = din("w_in", [D, INW])
    wr_d = din("w_ret", [2048, D])
    pl_d = din("pool_lin", [4, 256, 256])
    wp_d = din("w_pool", [D, D])
    wo_d = din("w_out", [D, D])
    nfin_d = din("norm_final", [1, D])
    cos_d = din("cosT", [128, SEQ])
    sin_d = din("sinT", [128, SEQ])
    cst_d = din("cst", [128, C_END])
    out_d = nc.dram_tensor("out", [BPC * SEQ, D], F32, kind="ExternalOutput").ap()
    TOT = 210944
    dbg_d = nc.dram_tensor("dbg", [128, TOT], U8, kind="ExternalOutput").ap() if stage else None

    def scr(name, shape):
        return nc.dram_tensor(name, list(shape), BF16, kind="Internal").ap()

    s13 = [scr("s13_0", [11, 128, 2, 8, 256]), scr("s13_1", [11, 128, 2, 8, 256])]
    s2 = [scr("s2_0", [8, 128, NS, 128]), scr("s2_1", [8, 128, NS, 128])]
    sin_s = scr("s_in", [18, 128, 8, 512])
    sr = scr("s_r", [4, 128, 16, 256])
    spl = scr("s_pl", [128, 4, 2, 256])
    sp_s = scr("s_p", [2, 128, 8, 512])
    so_s = scr("s_o", [2, 128, 8, 512])

    es = ExitStack()
    arena = es.enter_context(nc.sbuf_tensor("arena", [128, TOT], U8))
    pst = [es.enter_context(nc.psum_tensor(f"ps{i}", [128, 512], F32)) for i in range(8)]
    P = [PS(pst[i], i) for i in range(8)]

    def sb(off, shape, dt):
        return SB(arena, off, shape, dt)

    xT = sb(0, (8, 512), F32)
    stF = sb(16384, (4, 2, 512), F32)
    stB = sb(32768, (4, 2, 512), BF16)
    cosS = sb(40960, (SEQ,), F32)
    sinS = sb(49152, (SEQ,), F32)
    cst = sb(57344, (C_END,), F32)
    wfin = sb(64512, (D,), F32)
    hT = sb(68608, (8, 512), BF16)
    M0 = 76800
    identb = sb(M0, (128,), BF16)
    onesb = sb(M0 + 256, (128,), BF16)
    vecT = sb(M0 + 512, (128,), F32)
    cact = sb(M0 + 1024, (2, 8), F32)
    modT = sb(M0 + 1088, (2, 72), F32)
    Asc = sb(M0 + 1664, (3, 2, 8), F32)
    Gsc = sb(M0 + 1856, (3, 2, 8), F32)
    Uh = sb(M0 + 2048, (8, 16), F32)
    t16f = sb(M0 + 2560, (2, 16), F32)
    mv = sb(M0 + 2656, (4, 2), F32)
    rs4 = sb(M0 + 2688, (4,), F32)
    nb4 = sb(M0 + 2704, (4,), F32)
    sq4 = sb(M0 + 2720, (4,), F32)
    ssq = sb(M0 + 2736, (4,), F32)
    S_sb = sb(M0 + 3072, (4, 128), BF16)
    N0 = M0 + 4096
    st6h = [sb(N0 + h * 512, (6,), F32) for h in range(4)]
    mvh = [sb(N0 + 2048 + h * 512, (2,), F32) for h in range(4)]
    sc3 = [sb(N0 + 4096 + h * 512, (3,), F32) for h in range(4)]
    R0 = N0 + 6144
    ring_off = [R0 + i * RING_B for i in range(NRING)]
    A0 = R0 + NRING * RING_B
    assert A0 + 89 * 1024 <= TOT, (A0, TOT)

    def ar(off, shape, dt):
        return sb(A0 + off, shape, dt)

    KB = 1024
    sq = ar(0, (8, 512), BF16)
    rsd = ar(34 * KB, (512,), F32)
    tmpn = [ar(36 * KB, (512,), F32), ar(38 * KB, (512,), F32)]
    sa = [ar(8 * KB, (512,), F32), ar(10 * KB, (512,), F32)]
    hid = ar(12 * KB, (NS, 512), BF16)
    rtmp = [ar(i * 2 * KB, (512,), F32) for i in range(4)]
    on_t = ar(0, (4, 512), BF16)
    kd = ar(4 * KB, (1024,), BF16)
    qd = ar(6 * KB, (8, 128), BF16)
    qT = ar(8 * KB, (8, 512), BF16)
    kT = ar(16 * KB, (8, 512), BF16)
    v_tok = ar(24 * KB, (4, 2048), BF16)
    sgT = ar(40 * KB, (16, 512), BF16)
    gatedT = ar(56 * KB, (16, 512), BF16)
    sig_ar = ar(73 * KB, (8, 512), BF16)
    sig_ap = ar(81 * KB, (8, 512), BF16)
    m_r = ar(0, (8, 512), F32)
    ptmp = [ar(16 * KB, (2, 528), F32), ar(16 * KB + 4224, (2, 528), F32)]
    U = ar(24 * KB + 256, (8, 528), F32)
    pooledT = ar(41 * KB, (8, 512), BF16)
    mixp = ar(57 * KB, (8, 512), BF16)
    merged = ar(16 * KB, (8, 512), BF16)
    m_p = [ar(28 * KB, (512,), F32), ar(30 * KB, (512,), F32)]
    otok = [ar(40 * KB, (D,), F32), ar(44 * KB, (D,), F32)]
    junk = ar(36 * KB, (512,), BF16)
    xtok = [ar(64 * KB + i * 4 * KB, (D,), F32) for i in range(4)]
    xTn = ar(48 * KB, (8, 512), F32)
    adab = [ar(0, (8, 512), F32), ar(16 * KB, (8, 512), F32)]
    vec128 = ar(32 * KB, (128,), F32)
    c16s = SB(arena, A0 + 32 * KB + 512, (128,), F32, parts=16)

    identf = cst[C_ID:C_ID + 128]
    ring_i = [0]
    psi = [0]

    def ps_next():
        p = P[psi[0] % 7]
        psi[0] += 1
        return p

    def dma(queue, out_ap, in_ap, reads, writes, slot, **kw):
        def fn(e, sem):
            e.dma_start(out=out_ap, in_=in_ap, **kw).then_inc(sem, 16)
        return K.add(queue, fn, reads, writes, slot=slot)

    def dma_multi(queue, pairs, reads, writes, slot, **kw):
        def fn(e, sem):
            for (o, i) in pairs:
                e.dma_start(out=o, in_=i, **kw).then_inc(sem, 16)
        return K.add(queue, fn, reads, writes, slot=slot, ndma=len(pairs))

    def dref(name, blk=0):
        return Ref(None, [("d", name, blk)])

    w13v = [w13_d[l].rearrange("(dc p) f -> p dc f", p=128) for l in range(2)]
    w2v = [w2_d[l].rearrange("(s p) d -> p s d", p=128) for l in range(2)]
    winv = win_d.rearrange("(dc p) f -> p dc f", p=128)
    wrv = wr_d.rearrange("(ec p) d -> p ec d", p=128)
    plv = pl_d.rearrange("g (cc p) d -> p g cc d", p=128)
    wpv = wp_d.rearrange("(dc p) f -> p dc f", p=128)
    wov = wo_d.rearrange("(dc p) f -> p dc f", p=128)
    stg_off = [ring_off[0], ring_off[1], A0 + 73 * 1024, A0 + 81 * 1024]
    stg_list = [[0, 1]]
    stg_i = [0]
    fresh = [True]
    fresh_i = [0]

    def wsrc(name, blk):
        if name.startswith("s13_"):
            l = int(name[-1])
            return s13[l][blk], [((8, 256), w13v[l][:, :, blk * 256:(blk + 1) * 256], (0,)),
                                 ((8, 256), w13v[l][:, :, DFF + blk * 256:DFF + (blk + 1) * 256], (1,))]
        if name.startswith("s2_"):
            l = int(name[-1])
            src = w2v[l][:, :, blk * 128:(blk + 1) * 128]
            return s2[l][blk], [((11, 128), src[:, 0:11, :], (slice(0, 11),)), ((11, 128), src[:, 11:22, :], (slice(11, 22),))]
        if name in ("s_in", "s_p", "s_o"):
            srcv, scrv = {"s_in": (winv, sin_s), "s_p": (wpv, sp_s), "s_o": (wov, so_s)}[name]
            src = srcv[:, :, blk * 512:(blk + 1) * 512]
            return scrv[blk], [((4, 512), src[:, 0:4, :], (slice(0, 4),)), ((4, 512), src[:, 4:8, :], (slice(4, 8),))]
        if name == "s_r":
            src = wrv[:, :, blk * 256:(blk + 1) * 256]
            return sr[blk], [((8, 256), src[:, 0:8, :], (slice(0, 8),)), ((8, 256), src[:, 8:16, :], (slice(8, 16),))]
        if name == "s_pl":
            return spl, [((2, 2, 256), plv[:, 0:2], (slice(0, 2),)), ((2, 2, 256), plv[:, 2:4], (slice(2, 4),))]
        raise KeyError(name)

    def wload(name, blk, shape):
        scr_ap, halves = wsrc(name, blk)
        if fresh[0]:
            k = fresh_i[0] % 2
            fresh_i[0] += 1
            v = sb(ring_off[2 + k], shape, BF16)
            for hi, (hshape, src, vidx) in enumerate(halves):
                si = stg_list[0][stg_i[0] % len(stg_list[0])]
                stg_i[0] += 1
                st = sb(stg_off[si], hshape, F32)
                if name == "s_pl":
                    dma_multi("sp", [(st.full[:, g], src[:, g]) for g in range(2)], [], [st.all()], slot=f"stg{si}")
                else:
                    dma("sp", st.full, src, [], [st.all()], slot=f"stg{si}")
                K.copy("dve" if hi == 0 else "act", v[vidx], st.all())
            dma("pool", scr_ap, v.full, [v.all()], [dref(name, blk)], slot=f"wst{k}")
            return v
        i = ring_i[0] % NRING
        ring_i[0] += 1
        v = sb(ring_off[i], shape, BF16)
        dma("sp", v.full, scr_ap, [dref(name, blk)], [v.all()], slot=f"ring{i}")
        return v

    dma_multi("sp", [(cst.full, cst_d), (cosS.full, cos_d), (sinS.full, sin_d),
                     (vec128.full, vecs_d), (c16s.full, c16_d),
                     (wfin.full, nfin_d.partition_broadcast(128))],
              [], [cst.all(), cosS.all(), sinS.all(), vec128.all(), c16s.all(), wfin.all()], slot="cload")
    K.copy("dve", identb.all(), identf)
    K.memset("dve", onesb.all(), 1.0 / D)
    K.memset("dve", Uh.all(), 0.0)
    pv = ps_next()
    K.tr(pv.f(0, 128), vec128.all(), identf)
    K.copy("dve", vecT.all(), pv.f(0, 128))
    pc = ps_next()
    K.tr(pc.f(0, 16), c16s.all(), Ref(cst.full[0:16, C_ID:C_ID + 16], identf.gr))
    K.act(Ref(cact.full.rearrange("p a b -> p (a b)"), cact.all().gr), pc.f(0, 16), AF.Silu)
    adv = adaw_d.rearrange("(dc p) f -> p dc f", p=128)
    modrow = SB(arena, A0 + 33 * KB, (INW,), F32, parts=2)
    for blk in range(18):
        ab = adab[blk % 2]
        dma("sp", ab.full, adv[:, :, blk * 512:(blk + 1) * 512], [], [ab.all()], slot=f"ada{blk % 2}")
        pb_ = ps_next()
        prow = Ref(pb_.t[0:2, :], [("p", pb_.i)])
        for dc in range(8):
            K.mm(prow, Ref(cact.full[:, :, dc], cact.all().gr), ab[dc, :], dc == 0, dc == 7)
        K.copy("dve" if blk % 2 else "act", modrow[blk * 512:(blk + 1) * 512], prow)
    pm = ps_next()
    id2 = Ref(cst.full[0:2, C_ID:C_ID + 2], identf.gr)
    for j in range(72):
        K.tr(pm.f(2 * j, 2 * j + 2), modrow[j * 128:(j + 1) * 128], id2)
    pmv = pm.f(0, 144)
    for b in range(BPC):
        K.tt("dve", modT[b, :], Ref(pm.t[:, 0:144].rearrange("p (j b) -> p b j", b=2)[:, b, :], pmv.gr),
             vecT[0:72], ALU.add)
    NW = (72, 80, 88)
    for n in range(3):
        for b in range(BPC):
            K.stt("dve", Asc[n, b, :], modT[b, (3 * n + 1) * 8:(3 * n + 2) * 8], 1.0, vecT[NW[n]:NW[n] + 8],
                  ALU.add, ALU.mult)
            K.ts("dve", Gsc[n, b, :], modT[b, (3 * n + 2) * 8:(3 * n + 3) * 8], 0.5 if n != 1 else 1.0, None, ALU.mult)

    def shift(n, b, dc):
        return modT[b, 3 * n * 8 + dc:3 * n * 8 + dc + 1]

    pend_mm = []

    def flush_mm():
        while pend_mm:
            pend_mm.pop(0)()

    def x_updated(dc):
        K.act(sq[dc, :], xT[dc, :], AF.Square)
        flush_mm()
        pend_mm.append(lambda dc=dc: K.mm(P[7].f(), onesb.all(), sq[dc, :], dc == 0, dc == 7))

    def rms_mod(n, b):
        flush_mm()
        K.act(rsd.all(), P[7].f(), AF.Sqrt, bias=EPS)
        K.recip(rsd.all(), rsd.all())
        for dc in range(8):
            t = tmpn[dc % 2]
            K.tt("dve", t.all(), xT[dc, :], rsd.all(), ALU.mult)
            K.act(hT[dc, :], t.all(), AF.Identity, scale=Asc[n, b, dc:dc + 1], bias=shift(n, b, dc))

    def ffn(l, b, skip_norm=False, mid_hook=None, group_hook=None):
        n = 0 if l == 0 else 2
        if not skip_norm:
            rms_mod(n, b)
        for blk in range(11):
            w = wload(f"s13_{l}", blk, (2, 8, 256))
            for sl in range(2):
                s = 2 * blk + sl
                pa, pb = ps_next(), ps_next()
                for dc in range(8):
                    K.mm(pa.f(), w[0, dc, sl * 128:(sl + 1) * 128], hT[dc, :], dc == 0, dc == 7)
                for dc in range(8):
                    K.mm(pb.f(), w[1, dc, sl * 128:(sl + 1) * 128], hT[dc, :], dc == 0, dc == 7)
                t = sa[s % 2]
                K.act(t.all(), pa.f(), AF.Silu)
                K.tt("dve", hid[s, :], t.all(), pb.f(), ALU.mult)
        if mid_hook is not None:
            mid_hook()
        for ds in range(8):
            w = wload(f"s2_{l}", ds, (NS, 128))
            po = ps_next()
            for s in range(NS):
                K.mm(po.f(), w[s, :], hid[s, :], s == 0, s == NS - 1)
            K.stt("dve", xT[ds, :], po.f(), Gsc[n, b, ds:ds + 1], xT[ds, :], ALU.mult, ALU.add)
            if l == 0:
                x_updated(ds)
            if group_hook is not None:
                group_hook(ds)

    def proj_fm(blk_lo, nblk, consume):
        for bi in range(nblk):
            w = wload("s_in", blk_lo + bi, (8, 512))
            for sl in range(4):
                p = ps_next()
                for dc in range(8):
                    K.mm(p.f(), w[dc, sl * 128:(sl + 1) * 128], hT[dc, :], dc == 0, dc == 7)
                consume(bi * 4 + sl, p)

    mlim = [99]

    def mixer(b, tt):
        first = (tt == 0)
        pos0 = tt * T
        rms_mod(1, b)
        cosv = cosS[pos0:pos0 + T]
        sinv = sinS[pos0:pos0 + T]
        for (blk_lo, dst) in ((0, qT), (2, kT)):
            pend = {}

            def rope(si, p, dst=dst, pend=pend):
                if si % 2 == 0:
                    pend["x1"] = p
                    return
                p1, p2 = pend["x1"], p
                h = si // 2
                K.tt("dve", rtmp[0].all(), p1.f(), cosv, ALU.mult)
                K.tt("dve", rtmp[1].all(), p2.f(), sinv, ALU.mult)
                K.tt("pool", dst[2 * h, :], rtmp[0].all(), rtmp[1].all(), ALU.subtract)
                K.tt("dve", rtmp[2].all(), p1.f(), sinv, ALU.mult)
                K.tt("dve", rtmp[3].all(), p2.f(), cosv, ALU.mult)
                K.tt("pool", dst[2 * h + 1, :], rtmp[2].all(), rtmp[3].all(), ALU.add)
            proj_fm(blk_lo, 2, rope)
        if mlim[0] < 1:
            return
        for nb in range(4):
            w = wload("s_in", 4 + nb, (8, 512))
            for c in range(4):
                p = ps_next()
                for dc in range(8):
                    K.mm(p.f(), hT[dc, c * 128:(c + 1) * 128], w[dc, :], dc == 0, dc == 7)
                K.copy("act", v_tok[c, nb * 512:(nb + 1) * 512], p.f())
        def g_evac(si, p):
            K.act(sgT[si, :], p.f(), AF.Silu)
            K.ts("dve", sgT[si, :], sgT[si, :], vecT[104 + si:105 + si], None, ALU.mult)
        proj_fm(8, 4, g_evac)
        if mlim[0] < 2:
            return
        if first:
            K.memset("pool", stF.all(), 0.0)
            K.memset("pool", stB.all(), 0.0)
        fill_dst = {14: (sig_ar, 0), 15: (sig_ar, 4), 16: (sig_ap, 0), 17: (sig_ap, 4)}

        def filler_slices(blk):
            w = wload("s_in", blk, (8, 512))
            dst, base = fill_dst[blk]
            out = []
            for sl in range(4):
                def one(sl=sl, w=w, dst=dst, base=base):
                    p = ps_next()
                    for dc in range(8):
                        K.mm(p.f(), w[dc, sl * 128:(sl + 1) * 128], hT[dc, :], dc == 0, dc == 7)
                    K.act(dst[base + sl, :], p.f(), AF.Sigmoid)
                out.append(one)
            return out

        for c in range(4):
            cs = slice(c * 128, (c + 1) * 128)
            fl = filler_slices(14 + c)
            pk = ps_next()
            for fc in range(8):
                K.tr(pk.h(fc * 128, (fc + 1) * 128), kT[fc, cs], identb.all())
            for h in range(4):
                K.act(kd[h * 256:(h + 1) * 256], pk.h(h * 256, (h + 1) * 256), AF.Copy, scale=cst[C_KDEC + h:C_KDEC + h + 1])
            K.tt("pool", qd.all(), qT[:, cs],
                 Ref(cst.full[:, C_QDEC:C_QDEC + 1024].rearrange("p (a b) -> p a b", a=8), cst[C_QDEC:C_QDEC + 1024].gr),
                 ALU.mult)
            psc = ps_next()
            for h in range(4):
                for fi in range(2):
                    fc = 2 * h + fi
                    K.mm(psc.f(h * 128, (h + 1) * 128), kT[fc, cs], qT[fc, cs], fi == 0, fi == 1)
            K.tt("dve", Ref(S_sb.full.rearrange("p a b -> p (a b)"), S_sb.all().gr), psc.f(), cst[C_MASK:C_MASK + 512], ALU.mult)
            fl[0]()
            pos_ = []
            for h in range(4):
                po = ps_next()
                pos_.append(po)
                K.mm(po.f(), S_sb[h, :], v_tok[c, h * 512:(h + 1) * 512], True, False)
                for dc in range(2):
                    K.mm(po.f(), qd[2 * h + dc, :], stB[h, dc, :], False, dc == 1)

            def gn_tail(h, pos_=pos_):
                K.recip(sc3[h][1:2], sc3[h][0:1])
                K.stt("dve", sc3[h][2:3], mvh[h][0:1], -1.0, sc3[h][1:2], ALU.mult, ALU.mult)
                K.act(on_t[h, :], pos_[h].f(), AF.Identity, scale=sc3[h][1:2], bias=sc3[h][2:3])
            for h in range(4):
                K.add("dve", (lambda e, h=h, pp=pos_[h]: e.bn_stats(out=st6h[h].all().ap, in_=pp.f().ap)), [pos_[h].f()], [st6h[h].all()])
                K.add("dve", (lambda e, h=h: e.bn_aggr(out=mvh[h].all().ap, in_=st6h[h].all().ap)), [st6h[h].all()], [mvh[h].all()])
                K.act(sc3[h][0:1], mvh[h][1:2], AF.Sqrt, bias=EPS)
                if h >= 1:
                    gn_tail(h - 1)
            gn_tail(3)
            for h in range(4):
                for dc in range(2):
                    pu = ps_next()
                    K.mm(pu.f(), kd[h * 256 + dc * 128:h * 256 + (dc + 1) * 128], v_tok[c, h * 512:(h + 1) * 512], True, True)
                    K.stt("dve", stF[h, dc, :], stF[h, dc, :], cdec[h], pu.f(), ALU.mult, ALU.add)
                    K.copy("act", stB[h, dc, :], stF[h, dc, :])
            fl[1]()
            fl[2]()
            fl[3]()
            for h in range(4):
                pg = ps_next()
                for e4 in range(4):
                    K.tr(pg.h(e4 * 128, (e4 + 1) * 128), on_t[h, e4 * 128:(e4 + 1) * 128], identb.all())
                K.tt("dve", Ref(gatedT.full[:, h * 4:(h + 1) * 4, cs], gatedT[h * 4:(h + 1) * 4, cs].gr),
                     Ref(pg.t[:, :].bitcast(BF16)[:, 0:512].rearrange("p (a b) -> p a b", a=4), pg.h(0, 512).gr),
                     Ref(sgT.full[:, h * 4:(h + 1) * 4, cs], sgT[h * 4:(h + 1) * 4, cs].gr), ALU.mult)
        if mlim[0] < 3:
            return
        if first:
            K.memset("pool", Uh.all(), 0.0)
        K.copy("pool", U[:, 0:16], Uh.all())
        proj_fm(12, 2, lambda si, p: K.copy("act", U[si, 16:528], p.f()))
        K.copy("pool", Uh.all(), U[:, 512:528])
        dve_pool_ops = []
        for g, wdt in enumerate((2, 4, 8, 16)):
            srcgr = U[2 * g:2 * g + 2, :].gr
            cur_ap, cur_gr = U.full[:, 2 * g:2 * g + 2, :], srcgr
            lag = 1
            k = 0
            while lag < wdt:
                dst = ptmp[k % 2]
                k += 1
                K.tt("pool", Ref(dst.full[:, :, lag:528], dst.all().gr),
                     Ref(cur_ap[:, :, lag:528], cur_gr), Ref(cur_ap[:, :, 0:528 - lag], cur_gr), ALU.add)
                cur_ap, cur_gr = dst.full, dst.all().gr
                lag *= 2
            pooled = Ref(pooledT.full[:, 2 * g:2 * g + 2, :], pooledT[2 * g:2 * g + 2, :].gr)
            ug = Ref(U.full[:, 2 * g:2 * g + 2, 16:528], srcgr)
            K.stt("dve", pooled, Ref(cur_ap[:, :, 16:528], cur_gr), 1.0 / wdt, ug, ALU.mult, ALU.subtract)
            if first:
                rc = Ref(cst.full[:, C_RC + g * 16:C_RC + (g + 1) * 16].unsqueeze(1).broadcast_to([128, 2, 16]), cst[C_RC:C_END].gr)
                K.tt("dve", t16f.all(), Ref(cur_ap[:, :, 16:32], cur_gr), rc, ALU.mult)
                K.tt("dve", Ref(pooledT.full[:, 2 * g:2 * g + 2, 0:16], pooled.gr), t16f.all(),
                     Ref(U.full[:, 2 * g:2 * g + 2, 16:32], srcgr), ALU.subtract)
        for blk in range(4):
            w = wload("s_r", blk, (16, 256))
            for dl in range(2):
                ds = blk * 2 + dl
                p = ps_next()
                for ec in range(16):
                    K.mm(p.f(), w[ec, dl * 128:(dl + 1) * 128], gatedT[ec, :], ec == 0, ec == 15)
                K.tt("dve", m_r[ds, :], p.f(), sig_ar[ds, :], ALU.mult)
        if mlim[0] < 4:
            return
        wpl = wload("s_pl", 0, (4, 2, 256))
        for g in range(4):
            for dl in range(2):
                p = ps_next()
                for cc in range(2):
                    K.mm(p.f(), wpl[g, cc, dl * 128:(dl + 1) * 128], pooledT[2 * g + cc, :], cc == 0, cc == 1)
                K.act(mixp[2 * g + dl, :], p.f(), AF.Identity, scale=vecT[120 + 2 * g + dl:121 + 2 * g + dl])
        for blk in range(2):
            w = wload("s_p", blk, (8, 512))
            for dl in range(4):
                ds = blk * 4 + dl
                p = ps_next()
                for kc in range(8):
                    K.mm(p.f(), w[kc, dl * 128:(dl + 1) * 128], mixp[kc, :], kc == 0, kc == 7)
                t = m_p[ds % 2]
                K.tt("dve", t.all(), p.f(), sig_ap[ds, :], ALU.mult)
                K.tt("pool", merged[ds, :], t.all(), m_r[ds, :], ALU.add)
        for blk in range(2):
            w = wload("s_o", blk, (8, 512))
            for dl in range(4):
                ds = blk * 4 + dl
                p = ps_next()
                for kc in range(8):
                    K.mm(p.f(), w[kc, dl * 128:(dl + 1) * 128], merged[kc, :], kc == 0, kc == 7)
                K.stt("dve", xT[ds, :], p.f(), Gsc[1, b, ds:ds + 1], xT[ds, :], ALU.mult, ALU.add)
                x_updated(ds)

    def load_x(b, tt):
        r0 = b * SEQ + tt * T
        dma_multi("sp", [(xtok[c].full, x_d[r0 + c * 128:r0 + (c + 1) * 128, :]) for c in range(4)],
                  [], [xtok[c].all() for c in range(4)], slot="xload")

    def x_to_fm():
        for dc in range(8):
            p = ps_next()
            for c in range(4):
                K.tr(p.f(c * 128, (c + 1) * 128), xtok[c][dc * 128:(dc + 1) * 128], identf)
            K.copy("act" if dc % 2 else "dve", xT[dc, :], p.f())
            x_updated(dc)

    def prep_next_a():
        for dc in range(8):
            p = ps_next()
            for c in range(4):
                K.tr(p.f(c * 128, (c + 1) * 128), xtok[c][dc * 128:(dc + 1) * 128], identf)
            K.copy("act" if dc % 2 else "dve", xTn[dc, :], p.f())
            K.act(sq[dc, :], xTn[dc, :], AF.Square)
            pend_mm.append(lambda dc=dc: K.mm(P[7].f(), onesb.all(), sq[dc, :], dc == 0, dc == 7))

    def prep_next_b(ds, nb_):
        for _ in range(2):
            if pend_mm:
                pend_mm.pop(0)()
        if ds == 4:
            flush_mm()
            K.act(rsd.all(), P[7].f(), AF.Sqrt, bias=EPS)
            K.recip(rsd.all(), rsd.all())
            for dc in range(8):
                t = tmpn[dc % 2]
                K.tt("dve", t.all(), xTn[dc, :], rsd.all(), ALU.mult)
                K.act(hT[dc, :], t.all(), AF.Identity, scale=Asc[0, nb_, dc:dc + 1], bias=shift(0, nb_, dc))

    def adopt_next():
        for dc in range(8):
            K.copy("act" if dc % 2 else "dve", xT[dc, :], xTn[dc, :])

    def final_store(b, tt):
        r0 = b * SEQ + tt * T
        for c in range(4):
            cs = slice(c * 128, (c + 1) * 128)
            pf = [ps_next(), ps_next()]
            for dc in range(8):
                K.tr(pf[dc // 4].f((dc % 4) * 128, (dc % 4 + 1) * 128), xT[dc, cs], identf)
            for i in range(2):
                K.act(junk.all(), pf[i].f(), AF.Square, accum=ssq[i:i + 1])
            K.tt("dve", ssq[2:3], ssq[0:1], ssq[1:2], ALU.add)
            K.act(ssq[3:4], ssq[2:3], AF.Sqrt, scale=1.0 / D, bias=EPS)
            K.recip(ssq[3:4], ssq[3:4])
            ot = otok[c % 2]
            for i in range(2):
                K.stt("dve", ot[i * 512:(i + 1) * 512], pf[i].f(), ssq[3:4], wfin[i * 512:(i + 1) * 512], ALU.mult, ALU.mult)
            dma("pool", out_d[r0 + c * 128:r0 + (c + 1) * 128, :], ot.full, [ot.all()], [dref("out", r0 + c)], slot=f"ost{c % 2}")

    tiles = [(b, tt) for b in range(BPC) for tt in range(NT)]
    if ntiles:
        tiles = tiles[:ntiles]
    order = ["pro", "x", "ffn1", "mix", "ffn2", "final"]
    mlim[0] = 99
    if stage and stage.startswith("mix") and len(stage) >= 4:
        mlim[0] = "ABCDE".index(stage[3])
        if len(stage) > 4:
            mlim.append(int(stage[4:]))
        stage = "mix"
    lim = order.index(stage) if stage else 99
    if lim >= 1:
        load_x(*tiles[0])
    for ti, (b, tt) in enumerate(tiles):
        fresh[0] = (ti == 0)
        has_next = ti + 1 < len(tiles)
        pipelined = (lim >= 5)
        if lim >= 1 and (ti == 0 or not pipelined):
            x_to_fm()
        if lim >= 2:
            stg_list[0] = [0, 1, 2, 3]
            ffn(0, b, skip_norm=(pipelined and ti > 0))
        stg_list[0] = [0, 1]
        if lim >= 3:
            mixer(b, tt)
        if has_next and lim >= 1:
            load_x(*tiles[ti + 1])
        if lim >= 4:
            stg_list[0] = [0, 1, 3]
            if has_next and pipelined:
                nb_ = tiles[ti + 1][0]
                ffn(1, b, mid_hook=prep_next_a, group_hook=lambda ds, nb_=nb_: prep_next_b(ds, nb_))
            else:
                ffn(1, b)
        if lim >= 5:
            final_store(b, tt)
            if has_next:
                adopt_next()
    if stage:
        whole = Ref(arena[:, :], [("s", g) for g in range(0, TOT // GR)])
        dma("sp", dbg_d, arena[:, :], [whole], [dref("dbg")], slot="dbgst")

    K.finalize()
    sems = {e: es.enter_context(nc.semaphore("sem_" + e)) for e in K.INORDER}
    slot_sems = {s: es.enter_context(nc.semaphore("sl_" + s)) for s in K.slot_cnt}
    block = es.enter_context(nc.Block())
    stats = {}

    @block.sync
    def _(e):
        stats["sp"] = K.emit("sp", e, sems, slot_sems)
        for sl in ("ost0", "ost1", "dbgst"):
            if sl in slot_sems:
                e.wait_ge(slot_sems[sl], K.slot_cnt[sl])

    @block.tensor
    def _(e):
        stats["pe"] = K.emit("pe", e, sems, slot_sems)

    @block.scalar
    def _(e):
        stats["act"] = K.emit("act", e, sems, slot_sems)

    @block.vector
    def _(e):
        stats["dve"] = K.emit("dve", e, sems, slot_sems)

    @block.gpsimd
    def _(e):
        stats["pool"] = K.emit("pool", e, sems, slot_sems)

    es.close()
    build_program.stats = {k: (len(K.ops[k]), v) for k, v in stats.items()}
    return nc, (cosT_np, sinT_np, cst_np)


_CACHE = {}


def kernel(x, c, ada_w, ada_b, norm_ffn1, ffn1_w13, ffn1_w2, norm_mix, w_in, ret_gn_w, w_ret_branch, pool_lin,
           pool_scale, w_pool_branch, w_out, norm_ffn2, ffn2_w13, ffn2_w2, norm_final):
    f = lambda a: np.ascontiguousarray(np.asarray(a, dtype=np.float32))
    if "nc" not in _CACHE:
        _CACHE["nc"] = build_program()
    nc, (cosT, sinT, cst) = _CACHE["nc"]
    x = f(x)
    c = f(c)
    vecs = np.concatenate([f(ada_b).reshape(72, 128), f(norm_ffn1).reshape(8, 128), f(norm_mix).reshape(8, 128),
                           f(norm_ffn2).reshape(8, 128), f(norm_final).reshape(8, 128), f(ret_gn_w).reshape(16, 128),
                           f(pool_scale).reshape(8, 128)], axis=0)
    shared = {
        "vecs": np.ascontiguousarray(vecs),
        "ada_w": f(ada_w)[0], "ffn1_w13": f(ffn1_w13)[0], "ffn2_w13": f(ffn2_w13)[0],
        "ffn1_w2": f(ffn1_w2)[0], "ffn2_w2": f(ffn2_w2)[0], "w_in": f(w_in)[0], "w_ret": f(w_ret_branch)[0],
        "pool_lin": f(pool_lin)[0], "w_pool": f(w_pool_branch)[0], "w_out": f(w_out)[0],
        "norm_final": f(norm_final).reshape(1, D), "cosT": cosT, "sinT": sinT, "cst": cst,
    }
    in_maps = []
    for i in range(NCORES):
        m = dict(shared)
        m["x"] = np.ascontiguousarray(x[i * BPC:(i + 1) * BPC].reshape(BPC * SEQ, D))
        m["c16"] = np.ascontiguousarray(c[i * BPC:(i + 1) * BPC].reshape(BPC * 8, 128))
        in_maps.append(m)
    res = run_bass_kernel_spmd(nc, in_maps, core_ids=list(range(NCORES)))
    out = np.concatenate([np.asarray(r["out"]).reshape(BPC, SEQ, D) for r in res.results], axis=0)
    return out.astype(np.float32)
```
